# Optimizing a Trainium2 kernel written in Bass

```python
import math
import jax, jax.numpy as jnp
from jax import lax
import numpy as np

D_MODEL = 2048
BATCH = 4
SEQ = 4096
DEPTH = 4

D_FF = 4 * D_MODEL
NORM_EPS = 1e-6

MLA_HEADS = 8
MLA_Q_RANK = 512
MLA_KV_RANK = 512
MLA_NOPE_DIM = 128
MLA_ROPE_DIM = 64
MLA_V_DIM = 128
ROPE_THETA = 10000.0
Q_BLOCK = 128

MOBA_HEADS = 8
MOBA_HEAD_DIM = 128
MOBA_BLOCK = 256
MOBA_TOPK = 3
MOBA_Q_CHUNK = 16

DIL_WINDOWS = (128, 512, 2048)
DIL_RATES = (1, 4, 16)
N_DIL = 3
DIL_HEADS_PER_GROUP = 4
DIL_HEAD_DIM = 128

SWA_Q_HEADS = 16
SWA_KV_HEADS = 2
SWA_HEAD_DIM = 64
SWA_WINDOW = 128

BAND = 128

EVEN_IN = MLA_Q_RANK + MLA_KV_RANK + MLA_ROPE_DIM + 3 * MOBA_HEADS * MOBA_HEAD_DIM
EVEN_OUT = MLA_HEADS * MLA_V_DIM + MOBA_HEADS * MOBA_HEAD_DIM
ODD_IN = 3 * N_DIL * DIL_HEADS_PER_GROUP * DIL_HEAD_DIM + (SWA_Q_HEADS + 2 * SWA_KV_HEADS) * SWA_HEAD_DIM
ODD_OUT = DIL_HEADS_PER_GROUP * DIL_HEAD_DIM + SWA_Q_HEADS * SWA_HEAD_DIM

kernel_name = "hybrid_mla_moba_dilated_swa_trunk"


def rms_norm(x, g):
    xf = x.astype(jnp.float32)
    y = xf * lax.rsqrt(jnp.mean(xf * xf, axis=-1, keepdims=True) + NORM_EPS)
    return (y * g.astype(jnp.float32)).astype(x.dtype)


def alibi_slopes(n):
    return jnp.exp2(-8.0 * jnp.arange(1, n + 1, dtype=jnp.float32) / n)


def apply_rope(x, pos):
    half = x.shape[-1] // 2
    inv_freq = ROPE_THETA ** (-jnp.arange(half, dtype=jnp.float32) / half)
    ang = pos.astype(jnp.float32)[:, None] * inv_freq[None, :]
    shape = (1, x.shape[1]) + (1,) * (x.ndim - 3) + (half,)
    cos = jnp.cos(ang).reshape(shape)
    sin = jnp.sin(ang).reshape(shape)
    xf = x.astype(jnp.float32)
    x1, x2 = xf[..., :half], xf[..., half:]
    return jnp.concatenate([x1 * cos - x2 * sin, x2 * cos + x1 * sin], axis=-1).astype(x.dtype)


def mla_attention(q_lat, kv_lat, k_pe, q_norm, w_uq, kv_norm, w_ukv):
    B, S, _ = q_lat.shape
    H, dn, dr, dv = MLA_HEADS, MLA_NOPE_DIM, MLA_ROPE_DIM, MLA_V_DIM
    pos = jnp.arange(S, dtype=jnp.int32)
    q = (rms_norm(q_lat, q_norm) @ w_uq).reshape(B, S, H, dn + dr)
    kv = (rms_norm(kv_lat, kv_norm) @ w_ukv).reshape(B, S, H, dn + dv)
    q = jnp.concatenate([q[..., :dn], apply_rope(q[..., dn:], pos)], axis=-1)
    k_rot = jnp.broadcast_to(apply_rope(k_pe, pos)[:, :, None, :], (B, S, H, dr))
    k = jnp.concatenate([kv[..., :dn], k_rot], axis=-1)
    v = kv[..., dn:]
    scale = (dn + dr) ** -0.5

    def query_block(c):
        q0 = c * Q_BLOCK
        qc = lax.dynamic_slice_in_dim(q, q0, Q_BLOCK, axis=1)
        s = jnp.einsum("bqhd,bkhd->bhqk", qc, k, preferred_element_type=jnp.float32) * scale
        qpos = q0 + jnp.arange(Q_BLOCK, dtype=jnp.int32)
        s = jnp.where(pos[None, :] <= qpos[:, None], s, -jnp.inf)
        p = jax.nn.softmax(s, axis=-1)
        return jnp.einsum("bhqk,bkhd->bqhd", p.astype(v.dtype), v, preferred_element_type=jnp.float32)

    o = lax.map(query_block, jnp.arange(S // Q_BLOCK))
    return o.transpose(1, 0, 2, 3, 4).reshape(B, S, H * dv).astype(q_lat.dtype)


def moba_attention(q, k, v):
    B, S, H, dh = q.shape
    nb = -(-S // MOBA_BLOCK)
    pad = nb * MOBA_BLOCK - S
    kp = jnp.pad(k, ((0, 0), (0, pad), (0, 0), (0, 0)))
    vp = jnp.pad(v, ((0, 0), (0, pad), (0, 0), (0, 0)))
    kb = kp.reshape(B, nb, MOBA_BLOCK, H, dh).transpose(0, 3, 1, 2, 4)
    vb = vp.reshape(B, nb, MOBA_BLOCK, H, dh).transpose(0, 3, 1, 2, 4)
    kmean = jnp.mean(kb.astype(jnp.float32), axis=3).astype(q.dtype)
    k_sel = min(MOBA_TOPK, nb)
    slopes = alibi_slopes(H)
    scale = dh ** -0.5
    blk_ids = jnp.arange(nb, dtype=jnp.int32)
    offs = jnp.arange(MOBA_BLOCK, dtype=jnp.int32)
    b_ix = jnp.arange(B)[:, None, None, None]
    h_ix = jnp.arange(H)[None, :, None, None]

    def query_chunk(c):
        t0 = c * MOBA_Q_CHUNK
        qc = lax.dynamic_slice_in_dim(q, t0, MOBA_Q_CHUNK, axis=1).transpose(0, 2, 1, 3)
        tpos = t0 + jnp.arange(MOBA_Q_CHUNK, dtype=jnp.int32)
        own = t0 // MOBA_BLOCK
        gate = jnp.einsum("bhqd,bhnd->bhqn", qc, kmean, preferred_element_type=jnp.float32)
        gate = jnp.where(blk_ids < own, gate, -jnp.inf)
        gval, gidx = lax.top_k(gate, k_sel)
        own_idx = jnp.broadcast_to(own, gidx.shape[:-1] + (1,)).astype(gidx.dtype)
        idx = jnp.concatenate([gidx, own_idx], axis=-1)
        ok = jnp.concatenate([gval > -jnp.inf, jnp.ones(own_idx.shape, bool)], axis=-1)
        kg = kb[b_ix, h_ix, idx]
        vg = vb[b_ix, h_ix, idx]
        s = jnp.einsum("bhqd,bhqnjd->bhqnj", qc, kg, preferred_element_type=jnp.float32) * scale
        kpos = idx[..., None] * MOBA_BLOCK + offs
        dist = tpos[None, None, :, None, None] - kpos
        mask = ok[..., None] & (dist >= 0)
        s = jnp.where(mask, s - slopes[None, :, None, None, None] * dist.astype(jnp.float32), -jnp.inf)
        p = jax.nn.softmax(s.reshape(B, H, MOBA_Q_CHUNK, -1), axis=-1).reshape(s.shape)
        return jnp.einsum("bhqnj,bhqnjd->bqhd", p.astype(vg.dtype), vg, preferred_element_type=jnp.float32)

    o = lax.map(query_chunk, jnp.arange(S // MOBA_Q_CHUNK))
    return o.transpose(1, 0, 2, 3, 4).reshape(B, S, H * dh).astype(q.dtype)


def banded_window_attention(q, k, v, slopes, max_dist, pos_scale):
    N, L, Hk, G, dh = q.shape
    nb = -(-L // BAND)
    Lp = nb * BAND
    qp = jnp.pad(q, ((0, 0), (0, Lp - L), (0, 0), (0, 0), (0, 0))).reshape(N, nb, BAND, Hk, G, dh)
    kp = jnp.pad(k, ((0, 0), (BAND, Lp - L), (0, 0), (0, 0))).reshape(N, nb + 1, BAND, Hk, dh)
    vp = jnp.pad(v, ((0, 0), (BAND, Lp - L), (0, 0), (0, 0))).reshape(N, nb + 1, BAND, Hk, dh)
    kw = jnp.concatenate([kp[:, :-1], kp[:, 1:]], axis=2)
    vw = jnp.concatenate([vp[:, :-1], vp[:, 1:]], axis=2)
    s = jnp.einsum("nbqhgd,nbkhd->nbhgqk", qp, kw, preferred_element_type=jnp.float32) * (dh ** -0.5)
    blk = jnp.arange(nb, dtype=jnp.int32)[:, None, None]
    qpos = blk * BAND + jnp.arange(BAND, dtype=jnp.int32)[None, :, None]
    kpos = blk * BAND - BAND + jnp.arange(2 * BAND, dtype=jnp.int32)[None, None, :]
    dist = qpos - kpos
    mask = (dist >= 0) & (dist <= max_dist) & (kpos >= 0)
    bias = -(slopes[None, :, :, None, None] * (pos_scale * dist).astype(jnp.float32)[:, None, None])
    s = jnp.where(mask[:, None, None], s + bias, -jnp.inf)
    m = jnp.max(s, axis=-1)
    p = jnp.exp(s - m[..., None])
    l = jnp.sum(p, axis=-1)
    acc = jnp.einsum("nbhgqk,nbkhd->nbqhgd", p.astype(vw.dtype), vw, preferred_element_type=jnp.float32)
    acc = acc.reshape(N, Lp, Hk, G, dh)[:, :L]
    m = m.transpose(0, 1, 4, 2, 3).reshape(N, Lp, Hk, G)[:, :L]
    l = l.transpose(0, 1, 4, 2, 3).reshape(N, Lp, Hk, G)[:, :L]
    return acc, m, l


def dilated_attention(q, k, v):
    B, S, _, dh = q.shape
    HG = DIL_HEADS_PER_GROUP
    slopes = alibi_slopes(N_DIL * HG).reshape(N_DIL, HG)
    accs, ms, ls = [], [], []
    for g in range(N_DIL):
        w, d = DIL_WINDOWS[g], DIL_RATES[g]

        def strided(x):
            return x.reshape(B, S // d, d, HG, dh).transpose(0, 2, 1, 3, 4).reshape(B * d, S // d, HG, dh)

        sl = slice(g * HG, (g + 1) * HG)
        acc, m, l = banded_window_attention(strided(q[:, :, sl])[:, :, :, None], strided(k[:, :, sl]),
                                            strided(v[:, :, sl]), slopes[g][:, None], w // d, d)
        accs.append(acc.reshape(B, d, S // d, HG, dh).transpose(0, 2, 1, 3, 4).reshape(B, S, HG, dh))
        ms.append(m.reshape(B, d, S // d, HG).transpose(0, 2, 1, 3).reshape(B, S, HG))
        ls.append(l.reshape(B, d, S // d, HG).transpose(0, 2, 1, 3).reshape(B, S, HG))
    acc = jnp.stack(accs)
    m = jnp.stack(ms)
    l = jnp.stack(ls)
    wts = jnp.exp(m - jnp.max(m, axis=0, keepdims=True))
    out = jnp.sum(acc * wts[..., None], axis=0) / jnp.sum(l * wts, axis=0)[..., None]
    return out.reshape(B, S, HG * dh).astype(q.dtype)


def swa_sink_attention(q, k, v, sinks):
    B, S, Hq, dh = q.shape
    Hkv = k.shape[2]
    G = Hq // Hkv
    slopes = alibi_slopes(Hq).reshape(Hkv, G)
    acc, m, l = banded_window_attention(q.reshape(B, S, Hkv, G, dh), k, v, slopes, SWA_WINDOW - 1, 1)
    sk = sinks.astype(jnp.float32).reshape(Hkv, G)
    mx = jnp.maximum(m, sk)
    a = jnp.exp(m - mx)
    out = acc * a[..., None] / (l * a + jnp.exp(sk - mx))[..., None]
    return out.reshape(B, S, Hq * dh).astype(q.dtype)


def even_mixer(h, w_in, q_norm, w_uq, kv_norm, w_ukv, w_out):
    B, S, _ = h.shape
    hd = MOBA_HEADS * MOBA_HEAD_DIM
    sizes = [MLA_Q_RANK, MLA_KV_RANK, MLA_ROPE_DIM, hd, hd, hd]
    z = h @ w_in
    q_lat, kv_lat, k_pe, qb, kb, vb = jnp.split(z, np.cumsum(sizes)[:-1].tolist(), axis=-1)
    a = mla_attention(q_lat, kv_lat, k_pe, q_norm, w_uq, kv_norm, w_ukv)
    shp = (B, S, MOBA_HEADS, MOBA_HEAD_DIM)
    b = moba_attention(qb.reshape(shp), kb.reshape(shp), vb.reshape(shp))
    return jnp.concatenate([a, b], axis=-1).astype(h.dtype) @ w_out


def odd_mixer(h, w_in, sinks, w_out):
    B, S, _ = h.shape
    cd = N_DIL * DIL_HEADS_PER_GROUP * DIL_HEAD_DIM
    sizes = [cd, cd, cd, SWA_Q_HEADS * SWA_HEAD_DIM, SWA_KV_HEADS * SWA_HEAD_DIM, SWA_KV_HEADS * SWA_HEAD_DIM]
    z = h @ w_in
    qc, kc, vc, qd, kd, vd = jnp.split(z, np.cumsum(sizes)[:-1].tolist(), axis=-1)
    cshp = (B, S, N_DIL * DIL_HEADS_PER_GROUP, DIL_HEAD_DIM)
    c = dilated_attention(qc.reshape(cshp), kc.reshape(cshp), vc.reshape(cshp))
    kvshp = (B, S, SWA_KV_HEADS, SWA_HEAD_DIM)
    d = swa_sink_attention(qd.reshape(B, S, SWA_Q_HEADS, SWA_HEAD_DIM), kd.reshape(kvshp),
                           vd.reshape(kvshp), sinks)
    return jnp.concatenate([c, d], axis=-1).astype(h.dtype) @ w_out


def squared_relu_mlp(h, w_up, w_down):
    return jnp.square(jax.nn.relu(h @ w_up)) @ w_down


def setup_inputs(seed: int = 0) -> dict:
    key = jax.random.key(seed)
    ks = jax.random.split(key, 15)
    n_even = (DEPTH + 1) // 2
    n_odd = DEPTH // 2
    f32 = jnp.float32

    def w(k, shape, fan_in):
        return jax.random.normal(k, shape, f32) * fan_in ** -0.5

    def gain(k, shape):
        return 1.0 + 0.05 * jax.random.normal(k, shape, f32)

    return {
        "x": jax.random.normal(ks[0], (BATCH, SEQ, D_MODEL), f32),
        "attn_norm": gain(ks[1], (DEPTH, D_MODEL)),
        "mlp_norm": gain(ks[2], (DEPTH, D_MODEL)),
        "w_up": w(ks[3], (DEPTH, D_MODEL, D_FF), D_MODEL),
        "w_down": w(ks[4], (DEPTH, D_FF, D_MODEL), D_FF),
        "ev_w_in": w(ks[5], (n_even, D_MODEL, EVEN_IN), D_MODEL),
        "ev_q_norm": gain(ks[6], (n_even, MLA_Q_RANK)),
        "ev_w_uq": w(ks[7], (n_even, MLA_Q_RANK, MLA_HEADS * (MLA_NOPE_DIM + MLA_ROPE_DIM)), MLA_Q_RANK),
        "ev_kv_norm": gain(ks[8], (n_even, MLA_KV_RANK)),
        "ev_w_ukv": w(ks[9], (n_even, MLA_KV_RANK, MLA_HEADS * (MLA_NOPE_DIM + MLA_V_DIM)), MLA_KV_RANK),
        "ev_w_out": w(ks[10], (n_even, EVEN_OUT, D_MODEL), EVEN_OUT),
        "od_w_in": w(ks[11], (n_odd, D_MODEL, ODD_IN), D_MODEL),
        "od_sinks": 0.5 * jax.random.normal(ks[12], (n_odd, SWA_Q_HEADS), f32),
        "od_w_out": w(ks[13], (n_odd, ODD_OUT, D_MODEL), ODD_OUT),
        "final_norm": gain(ks[14], (D_MODEL,)),
    }


def reference(x, attn_norm, mlp_norm, w_up, w_down, ev_w_in, ev_q_norm, ev_w_uq, ev_kv_norm,
              ev_w_ukv, ev_w_out, od_w_in, od_sinks, od_w_out, final_norm):
    for layer in range(DEPTH):
        i = layer // 2
        h = rms_norm(x, attn_norm[layer])
        if layer % 2 == 0:
            mix = even_mixer(h, ev_w_in[i], ev_q_norm[i], ev_w_uq[i], ev_kv_norm[i], ev_w_ukv[i], ev_w_out[i])
        else:
            mix = odd_mixer(h, od_w_in[i], od_sinks[i], od_w_out[i])
        x = x + mix.astype(x.dtype)
        h = rms_norm(x, mlp_norm[layer])
        x = x + squared_relu_mlp(h, w_up[layer], w_down[layer]).astype(x.dtype)
    return rms_norm(x, final_norm)
```

```python
import numpy as np
import concourse.bass as bass
import concourse.mybir as mybir
from contextlib import ExitStack

F32 = mybir.dt.float32
BF16 = mybir.dt.bfloat16
ALU = mybir.AluOpType
AF = mybir.ActivationFunctionType
AX = mybir.AxisListType

ENGS = ("pe", "act", "dve", "pool", "sp")


class Buf:
    def __init__(self, name, t):
        self.name = name
        self.t = t
        self.st = {}

    def __getitem__(self, idx):
        return self.t[idx]

    def view(self, name, ap):
        v = Buf(name, ap)
        v.st = self.st
        return v


class Op:
    __slots__ = ("eng", "fn", "reads", "writes", "dma", "chan", "deps", "sig", "ordv")

    def __init__(self, eng, fn, reads, writes, dma=False, chan=None):
        self.eng = eng
        self.fn = fn
        self.reads = reads
        self.writes = writes
        self.dma = dma
        self.chan = chan
        self.deps = ()
        self.sig = False
        self.ordv = 0


def _norm(lst):
    out = []
    for x in lst:
        if isinstance(x, tuple):
            out.append(x)
        else:
            out.append((x, None))
    return out


class Prog:
    def __init__(self, nc, same_engine_raw=True):
        self.nc = nc
        self.ops = []
        self.es = ExitStack()
        self.same_engine_raw = same_engine_raw
        self.engobj = {"pe": nc.tensor, "act": nc.scalar, "dve": nc.vector,
                       "pool": nc.gpsimd, "sp": nc.sync}
        self.nbuf = 0

    def sbuf(self, name, shape, dt):
        t = self.es.enter_context(self.nc.sbuf_tensor(name, list(shape), dt))
        return Buf(name, t)

    def psum(self, name, shape, dt):
        t = self.es.enter_context(self.nc.psum_tensor(name, list(shape), dt))
        return Buf(name, t)

    def dram(self, name, shape, dt, kind="Internal"):
        t = self.nc.dram_tensor(name, list(shape), dt, kind=kind)
        return Buf(name, t.ap())

    def op(self, eng, fn, reads=(), writes=()):
        self.ops.append(Op(eng, fn, _norm(reads), _norm(writes)))

    def dma(self, q, out_ap, in_ap, reads=(), writes=(), chan=None, **kw):
        def fn(e, out_ap=out_ap, in_ap=in_ap, kw=kw):
            return e.dma_start(out=out_ap, in_=in_ap, **kw)
        writes = _norm(writes)
        if chan is None:
            chan = "%s:%s" % (writes[0][0].name, writes[0][1])
        self.ops.append(Op(q, fn, _norm(reads), writes, dma=True, chan=chan))

    @staticmethod
    def _states(buf, key):
        st = buf.st
        if key is None:
            if None not in st:
                st[None] = [None, {}]
            return list(st.values())
        res = []
        if None in st:
            res.append(st[None])
        if key not in st:
            st[key] = [None, {}]
        res.append(st[key])
        return res

    def _analyze(self):
        for i, o in enumerate(self.ops):
            deps = set()
            ek = ("dma", o.chan) if o.dma else o.eng
            for (b, k) in o.reads:
                for s in self._states(b, k):
                    if s[0] is not None:
                        deps.add(s[0])
            for (b, k) in o.writes:
                for s in self._states(b, k):
                    if s[0] is not None:
                        deps.add(s[0])
                    deps.update(s[1].values())
            deps.discard(i)
            for (b, k) in o.reads:
                if k is None:
                    for s in self._states(b, None):
                        s[1][ek] = i
                else:
                    self._states(b, k)[-1][1][ek] = i
            for (b, k) in o.writes:
                if k is None:
                    b.st.clear()
                    b.st[None] = [i, {}]
                else:
                    s = self._states(b, k)[-1]
                    s[0] = i
                    s[1] = {}
            o.deps = deps

    def emit(self):
        nc = self.nc
        self._analyze()
        ops = self.ops
        for i, o in enumerate(ops):
            per = {}
            for d in o.deps:
                od = ops[d]
                if od.dma:
                    key = ("dma", od.chan)
                else:
                    if od.eng == o.eng and not o.dma:
                        if od.eng == "pe" or not self.same_engine_raw:
                            continue
                        raw = any((b.st is rb.st and (k is None or rk is None or k == rk))
                                  for (b, k) in od.writes for (rb, rk) in o.reads)
                        if not raw:
                            continue
                    key = ("eng", od.eng)
                if key not in per or per[key] < d:
                    per[key] = d
            o.deps = sorted(per.values())
            for d in o.deps:
                ops[d].sig = True
        cnt = {e: 0 for e in ENGS}
        for o in ops:
            if o.dma:
                continue
            if o.sig:
                cnt[o.eng] += 1
                o.ordv = cnt[o.eng]
        sems = {e: self.es.enter_context(nc.semaphore("s_" + e)) for e in ENGS}
        chans = {}
        chan_total = {}
        known = {e: {} for e in ENGS}
        nwait = 0
        for i, o in enumerate(ops):
            e = self.engobj[o.eng]
            kn = known[o.eng]
            for d in o.deps:
                od = ops[d]
                if od.dma:
                    key = ("dma", od.chan)
                    val = chan_total[od.chan]
                    sem = chans[od.chan]
                else:
                    key = ("eng", od.eng)
                    val = od.ordv
                    sem = sems[od.eng]
                if kn.get(key, 0) >= val:
                    continue
                e.wait_ge(sem, val)
                nwait += 1
                kn[key] = val
            ins = o.fn(e)
            if o.dma:
                ch = o.chan
                if ch not in chans:
                    chans[ch] = self.es.enter_context(nc.semaphore("d_" + str(ch)))
                    chan_total[ch] = 0
                chan_total[ch] += 16
                ins.then_inc(chans[ch], 16)
            elif o.sig:
                ins.then_inc(sems[o.eng], 1)
        for ch, tot in chan_total.items():
            self.engobj["sp"].wait_ge(chans[ch], tot)
        self.stats = dict(n_ops=len(ops), n_wait=nwait, n_chan=len(chans),
                          sig={e: cnt[e] for e in ENGS})
        return self.stats

    def close(self):
        self.es.close()


D = 2048
DFF = 8192
TB = 1024
TC = 512
EPS = 1e-6
NEG = -30000.0


class LCtx:
    def __init__(self, p):
        self.p = p
        self.xs = p.sbuf("xs", [128, 16, TB], F32)
        self.hT = p.sbuf("hT", [128, 16, TB], BF16)
        self.wA = [p.sbuf("wA%d" % i, [128, 16, 512], BF16) for i in range(2)]
        self.wB = [p.sbuf("wB%d" % i, [128, 4, 2048], BF16) for i in range(2)]
        self.uT = p.sbuf("uT", [128, 4, TB], BF16)
        self.sq = [p.sbuf("sq%d" % i, [128, 512], BF16) for i in range(2)]
        self.rstd = p.sbuf("rstd", [128, 512], F32)
        self.ones = p.sbuf("ones", [128, 128], BF16)
        self.stg = [p.sbuf("stg%d" % i, [128, 1024], BF16) for i in range(2)]
        self.stgf = [p.sbuf("stgf%d" % i, [128, 512], F32) for i in range(2)]
        self.gains = p.sbuf("gains", [128, 64], F32)
        self.ps = [p.psum("psA%d" % i, [128, 512], F32) for i in range(4)]
        self.psN = p.psum("psN", [128, 512], F32)
        self.psY = [p.psum("psY%d" % i, [128, 512], F32) for i in range(2)]
        self.nps = 0
        self.nY = 0
        self.nw = 0
        self.nsq = 0
        self.nstg = 0
        self.nalt = 0
        self.lat = self.wB[0].view("lat", self.wB[0].t[:, :, :].bitcast(F32))
        self.latn = p.sbuf("latn", [128, 4, TB], BF16)
        self.ropeb = p.sbuf("ropeb", [128, 1, TB], F32)
        self.cosT = p.sbuf("cosT", [128, TB], F32)
        self.sinT = p.sbuf("sinT", [128, TB], F32)
        p.op("dve", lambda e: e.memset(self.ones[:, :], 1.0), writes=[self.ones])

    def next_ps(self):
        b = self.ps[self.nps % 4]
        self.nps += 1
        return b

    def next_psY(self):
        b = self.psY[self.nY % 2]
        self.nY += 1
        return b

    def next_stg(self):
        b = self.stg[self.nstg % 2]
        self.nstg += 1
        return b


def load_gain(c, g_dram, col0, nk):
    p = c.p
    p.dma("sp", c.gains[:, col0:col0 + nk], g_dram[:, :], reads=[g_dram], writes=[(c.gains, col0)],
          chan="gains%d" % col0)


def rmsnorm_fm(c, src, nk, gcol, dst, dfeat, ntc, src_key=None):
    p = c.p
    for tc in range(ntc):
        ts = slice(tc * TC, (tc + 1) * TC)
        for k in range(nk):
            sq = c.sq[c.nsq % 2]
            c.nsq += 1
            p.op("act", lambda e, sq=sq, k=k, ts=ts: e.activation(sq[:, :], src[:, k, ts], AF.Square),
                 reads=[src], writes=[sq])
            p.op("pe", lambda e, sq=sq, k=k: e.matmul(c.psN[:, :], c.ones[:, :], sq[:, :], start=(k == 0), stop=(k == nk - 1)),
                 reads=[sq, c.ones], writes=[c.psN])
        p.op("dve", lambda e: e.tensor_scalar(c.rstd[:, :], c.psN[:, :], 1.0 / dfeat, EPS, ALU.mult, ALU.add),
             reads=[c.psN], writes=[c.rstd])
        p.op("act", lambda e: e.activation(c.rstd[:, :], c.rstd[:, :], AF.Sqrt),
             reads=[c.rstd], writes=[c.rstd])
        p.op("dve", lambda e: e.reciprocal(c.rstd[:, :], c.rstd[:, :]),
             reads=[c.rstd], writes=[c.rstd])
        for k in range(nk):
            p.op("dve", lambda e, k=k, ts=ts: e.scalar_tensor_tensor(
                dst[:, k, ts], src[:, k, ts], c.gains[:, gcol + k:gcol + k + 1], c.rstd[:, :], ALU.mult, ALU.mult),
                reads=[src, c.rstd, c.gains], writes=[(dst, ("n", k, tc))])


def load_w_slab(c, wbuf, w_dram, r0, nk, c0, ncols):
    p = c.p
    src = w_dram.t[r0:r0 + nk * 128, c0:c0 + ncols].rearrange("(k p) n -> p k n", p=128)
    p.dma("pool", wbuf[:, 0:nk, 0:ncols], src, reads=[w_dram], writes=[wbuf], chan=wbuf.name)


def lin_fm(c, act, nk, ntc, w_dram, r0, c0, ncols, evac, act_keys=None):
    p = c.p
    done = 0
    while done < ncols:
        n = min(512, ncols - done)
        wb = c.wA[c.nw % 2]
        c.nw += 1
        load_w_slab(c, wb, w_dram, r0, nk, c0 + done, n)
        for jj in range(n // 128):
            j = (done // 128) + jj
            for tc in range(ntc):
                ps = c.next_ps()
                for k in range(nk):
                    p.op("pe", lambda e, ps=ps, wb=wb, k=k, jj=jj, tc=tc: e.matmul(
                        ps[:, :], wb[:, k, jj * 128:(jj + 1) * 128], act[:, k, tc * TC:(tc + 1) * TC],
                        start=(k == 0), stop=(k == nk - 1)),
                        reads=[wb, act], writes=[ps])
                evac(j, tc, ps)
        done += n


def lin_tm(c, act, nk, ntt, w_dram, r0, c0, ncols, evac):
    p = c.p
    done = 0
    while done < ncols:
        n = min(512, ncols - done)
        wb = c.wA[c.nw % 2]
        c.nw += 1
        load_w_slab(c, wb, w_dram, r0, nk, c0 + done, n)
        for tt in range(ntt):
            ps = c.next_ps()
            for k in range(nk):
                p.op("pe", lambda e, ps=ps, wb=wb, k=k, tt=tt, n=n: e.matmul(
                    ps[:, 0:n], act[:, k, tt * 128:(tt + 1) * 128], wb[:, k, 0:n],
                    start=(k == 0), stop=(k == nk - 1)),
                    reads=[wb, act], writes=[ps])
            evac(tt, done, n, ps)
        done += n


def out_proj(c, w_out, natt):
    p = c.p

    def evac(j, tc, ps):
        ts = slice(tc * TC, (tc + 1) * TC)
        p.op("dve", lambda e: e.tensor_tensor(c.xs[:, j, ts], ps[:, :], c.xs[:, j, ts], ALU.add),
             reads=[ps, (c.xs, (j, tc))], writes=[(c.xs, (j, tc))])
    lin_fm(c, c.hT, natt, TB // TC, w_out, 0, 0, D, evac)


def mlp(c, w_up, w_dn):
    p = c.p
    ntc = TB // TC
    for g in range(DFF // 512):
        wu = c.wA[c.nw % 2]
        wd = c.wB[c.nw % 2]
        c.nw += 1
        load_w_slab(c, wu, w_up, 0, 16, g * 512, 512)
        srcd = w_dn.t[g * 512:(g + 1) * 512, :].rearrange("(k p) n -> p k n", p=128)
        p.dma("pool", wd[:, :, :], srcd, reads=[w_dn], writes=[wd], chan=wd.name)
        for tc in range(ntc):
            ts = slice(tc * TC, (tc + 1) * TC)
            for hc in range(4):
                ps = c.next_ps()
                for k in range(16):
                    p.op("pe", lambda e, ps=ps, wu=wu, k=k, hc=hc, ts=ts: e.matmul(
                        ps[:, :], wu[:, k, hc * 128:(hc + 1) * 128], c.hT[:, k, ts],
                        start=(k == 0), stop=(k == 15)),
                        reads=[wu, c.hT], writes=[ps])
                sf = c.stgf[hc % 2]
                p.op("act", lambda e, ps=ps, sf=sf: e.activation(sf[:, :], ps[:, :], AF.Relu),
                     reads=[ps], writes=[sf])
                p.op("dve", lambda e, sf=sf, hc=hc, ts=ts: e.tensor_tensor(c.uT[:, hc, ts], sf[:, :], sf[:, :], ALU.mult),
                     reads=[sf], writes=[(c.uT, (hc, tc))])
            for o in range(16):
                ps = c.next_psY()
                for hc in range(4):
                    p.op("pe", lambda e, ps=ps, wd=wd, hc=hc, o=o, ts=ts: e.matmul(
                        ps[:, :], wd[:, hc, o * 128:(o + 1) * 128], c.uT[:, hc, ts],
                        start=(hc == 0), stop=(hc == 3)),
                        reads=[wd, (c.uT, (hc, tc))], writes=[ps])
                p.op("dve", lambda e, ps=ps, o=o, ts=ts: e.tensor_tensor(c.xs[:, o, ts], ps[:, :], c.xs[:, o, ts], ALU.add),
                     reads=[ps, (c.xs, (o, tc))], writes=[(c.xs, (o, tc))])


EVEN_SPEC = [("lat", "qlat", 512, 1.0), ("lat", "kvlat", 512, 1.0),
             ("rope", "kr2", 128, 1.0),
             ("fm", "bq", 1024, 128 ** -0.5), ("fm", "bk", 1024, 1.0), ("tm", "bv", 1024, 1.0)]
ODD_SPEC = [("fm", "dq", 1536, 128 ** -0.5), ("fm", "dk", 1536, 1.0), ("tm", "dv", 1536, 1.0),
            ("fm", "sq", 1024, 64 ** -0.5), ("fm", "sk2", 256, 1.0), ("tm", "sv", 128, 1.0)]
UQ_SPEC = [("fm", "qn", 1024, 192 ** -0.5), ("rope", "qr", 512, 192 ** -0.5)]
UKV_SPEC = [("fm", "kn", 1024, 1.0), ("tm", "mv", 1024, 1.0)]


def spec_cols(spec):
    return sum(n * (2 if k == "rope" else 1) for (k, _, n, _) in spec)


def run_spec(c, spec, act, nk, w_dram, outs, tok0, lat=None, cs=None, col0=0):
    p = c.p
    ntc = TB // TC
    col = col0
    for (kind, name, ncols, scale) in spec:
        if kind == "fm":
            od = outs[name]

            def evac(j, tc, ps, od=od, scale=scale, name=name):
                st = c.next_stg()
                p.op("act", lambda e: e.activation(st[:, 0:TC], ps[:, :], AF.Copy, scale=scale),
                     reads=[ps], writes=[st])
                p.dma("sp", od.t[j * 128:(j + 1) * 128, tok0 + tc * TC: tok0 + (tc + 1) * TC], st[:, 0:TC],
                      reads=[st], writes=[(od, (j, tok0, tc))], chan=st.name)
            lin_fm(c, act, nk, ntc, w_dram, 0, col, ncols, evac)
            col += ncols
        elif kind == "lat":
            lb = lat

            def evac(j, tc, ps, lb=lb):
                p.op("act", lambda e: e.activation(lb[:, j, tc * TC:(tc + 1) * TC], ps[:, :], AF.Copy),
                     reads=[ps], writes=[(lb, (j, tc))])
            lin_fm(c, act, nk, ntc, w_dram, 0, col, ncols, evac)
            col += ncols
        elif kind == "rope":
            od = outs[name]
            nch = ncols // 128
            ropeb = c.ropeb

            def evac(j, tc, ps, od=od, scale=scale, nch=nch):
                ts = slice(tc * TC, (tc + 1) * TC)
                if j % 2 == 0:
                    p.op("dve", lambda e: e.tensor_tensor(ropeb[:, 0, ts], ps[:, :], cs[0][:, ts], ALU.mult),
                         reads=[ps, cs[0]], writes=[(ropeb, tc)])
                else:
                    jj = j // 2
                    sf = c.stgf[jj % 2]
                    p.op("dve", lambda e: e.tensor_tensor(sf[:, :], ps[:, :], cs[1][:, ts], ALU.mult),
                         reads=[ps, cs[1]], writes=[sf])
                    p.op("dve", lambda e: e.tensor_tensor(sf[:, :], sf[:, :], ropeb[:, 0, ts], ALU.add),
                         reads=[sf, (ropeb, tc)], writes=[sf])
                    st = c.next_stg()
                    p.op("act", lambda e: e.activation(st[:, 0:TC], sf[:, :], AF.Copy, scale=scale),
                         reads=[sf], writes=[st])
                    p.dma("sp", od.t[jj * 128:(jj + 1) * 128, tok0 + tc * TC: tok0 + (tc + 1) * TC], st[:, 0:TC],
                          reads=[st], writes=[(od, (jj, tok0, tc))], chan=st.name)
            lin_fm(c, act, nk, ntc, w_dram, 0, col, 2 * ncols, evac)
            col += 2 * ncols
        elif kind == "tm":
            od = outs[name]

            def evac(tt, coff, n, ps, od=od):
                st = c.next_stg()
                p.op("act", lambda e: e.activation(st[:, 0:n], ps[:, 0:n], AF.Copy),
                     reads=[ps], writes=[st])
                p.dma("sp", od.t[tok0 + tt * 128: tok0 + (tt + 1) * 128, coff:coff + n], st[:, 0:n],
                      reads=[st], writes=[(od, (tt, tok0, coff))], chan=st.name)
            lin_tm(c, act, nk, TB // 128, w_dram, 0, col, ncols, evac)
            col += ncols
    return col


EVEN_A1 = [("lat", "qlat", 512, 1.0)]
EVEN_A2 = [("lat", "kvlat", 512, 1.0)]
EVEN_A3 = [("rope", "kr2", 128, 1.0),
           ("fm", "bq", 1024, 128 ** -0.5), ("fm", "bk", 1024, 1.0), ("tm", "bv", 1024, 1.0)]
W_IN_EVEN_COLS = 512 + 512 + 256 + 3072
W_IN_ODD_COLS = 1536 * 3 + 1024 + 256 + 128
OUT_SHAPES_EVEN = {"qn": ("fm", 1024), "qr": ("fm", 512), "kn": ("fm", 1024), "kr2": ("fm", 128), "mv": ("tm", 1024),
                   "bq": ("fm", 1024), "bk": ("fm", 1024), "bv": ("tm", 1024)}
OUT_SHAPES_ODD = {"dq": ("fm", 1536), "dk": ("fm", 1536), "dv": ("tm", 1536),
                  "sq": ("fm", 1024), "sk2": ("fm", 256), "sv": ("tm", 128)}
TCORE = 2048


def build_L(i, nblk=TCORE // TB):
    nc = bass.Bass("TRN2", target_bir_lowering=False)
    p = Prog(nc)
    c = LCtx(p)
    T = TCORE
    xT = p.dram("xT", [D, T], F32, kind="ExternalInput")
    if i > 0:
        natt = 16 if (i - 1) % 2 == 0 else 12
        atT = p.dram("atT", [natt * 128, T], BF16, kind="ExternalInput")
        w_out = p.dram("w_out", [natt * 128, D], F32, kind="ExternalInput")
        g_mlp = p.dram("g_mlp", [128, 16], F32, kind="ExternalInput")
        w_up = p.dram("w_up", [D, DFF], F32, kind="ExternalInput")
        w_dn = p.dram("w_dn", [DFF, D], F32, kind="ExternalInput")
        load_gain(c, g_mlp, 0, 16)
    outs = {}
    if i < 4:
        even = (i % 2 == 0)
        g_att = p.dram("g_att", [128, 16], F32, kind="ExternalInput")
        load_gain(c, g_att, 16, 16)
        w_in = p.dram("w_in", [D, W_IN_EVEN_COLS if even else W_IN_ODD_COLS], F32, kind="ExternalInput")
        shapes = OUT_SHAPES_EVEN if even else OUT_SHAPES_ODD
        for nm, (kind, n) in shapes.items():
            outs[nm] = p.dram("o_" + nm, [n, T] if kind == "fm" else [T, n], BF16, kind="ExternalOutput")
        if even:
            g_q = p.dram("g_q", [128, 4], F32, kind="ExternalInput")
            g_kv = p.dram("g_kv", [128, 4], F32, kind="ExternalInput")
            load_gain(c, g_q, 32, 4)
            load_gain(c, g_kv, 36, 4)
            w_uq = p.dram("w_uq", [512, 2048], F32, kind="ExternalInput")
            w_ukv = p.dram("w_ukv", [512, 2048], F32, kind="ExternalInput")
            cosd = p.dram("cosd", [128, T], F32, kind="ExternalInput")
            sind = p.dram("sind", [128, T], F32, kind="ExternalInput")
        xo = p.dram("xo", [D, T], F32, kind="ExternalOutput")
    else:
        g_fin = p.dram("g_fin", [128, 16], F32, kind="ExternalInput")
        load_gain(c, g_fin, 16, 16)
        yo = p.dram("yo", [D, T], F32, kind="ExternalOutput")
    ntc = TB // TC
    for blk in range(nblk):
        tok0 = blk * TB
        tsl = slice(tok0, tok0 + TB)
        for k4 in range(4):
            p.dma("sp", c.xs[:, 4 * k4:4 * k4 + 4, :], xT.t[k4 * 512:(k4 + 1) * 512, tsl].rearrange("(k p) t -> p k t", p=128),
                  reads=[xT], writes=[c.xs], chan="xs")
        if i > 0:
            p.dma("sp", c.hT[:, 0:natt, :], atT.t[:, tsl].rearrange("(k p) t -> p k t", p=128),
                  reads=[atT], writes=[c.hT], chan="hT")
            out_proj(c, w_out, natt)
            rmsnorm_fm(c, c.xs, 16, 0, c.hT, D, ntc)
            mlp(c, w_up, w_dn)
        if i < 4:
            rmsnorm_fm(c, c.xs, 16, 16, c.hT, D, ntc)
            if even:
                p.dma("sp", c.cosT[:, :], cosd.t[:, tsl], reads=[cosd], writes=[c.cosT], chan="cosT")
                p.dma("sp", c.sinT[:, :], sind.t[:, tsl], reads=[sind], writes=[c.sinT], chan="sinT")
                cs = (c.cosT, c.sinT)
                col = run_spec(c, EVEN_A1, c.hT, 16, w_in, outs, tok0, lat=c.lat, col0=0)
                rmsnorm_fm(c, c.lat, 4, 32, c.latn, 512, ntc)
                run_spec(c, UQ_SPEC, c.latn, 4, w_uq, outs, tok0, cs=cs)
                col = run_spec(c, EVEN_A2, c.hT, 16, w_in, outs, tok0, lat=c.lat, col0=col)
                rmsnorm_fm(c, c.lat, 4, 36, c.latn, 512, ntc)
                run_spec(c, UKV_SPEC, c.latn, 4, w_ukv, outs, tok0)
                run_spec(c, EVEN_A3, c.hT, 16, w_in, outs, tok0, cs=cs, col0=col)
            else:
                run_spec(c, ODD_SPEC, c.hT, 16, w_in, outs, tok0)
            for k4 in range(4):
                p.dma("sp", xo.t[k4 * 512:(k4 + 1) * 512, tsl].rearrange("(k p) t -> p k t", p=128), c.xs[:, 4 * k4:4 * k4 + 4, :],
                      reads=[c.xs], writes=[(xo, (blk, k4))], chan="xs_st")
        else:
            for tc in range(ntc):
                ts = slice(tc * TC, (tc + 1) * TC)
                for k in range(16):
                    sq = c.sq[c.nsq % 2]
                    c.nsq += 1
                    p.op("act", lambda e, sq=sq, k=k, ts=ts: e.activation(sq[:, :], c.xs[:, k, ts], AF.Square),
                         reads=[c.xs], writes=[sq])
                    p.op("pe", lambda e, sq=sq, k=k: e.matmul(c.psN[:, :], c.ones[:, :], sq[:, :], start=(k == 0), stop=(k == 15)),
                         reads=[sq, c.ones], writes=[c.psN])
                p.op("dve", lambda e: e.tensor_scalar(c.rstd[:, :], c.psN[:, :], 1.0 / D, EPS, ALU.mult, ALU.add),
                     reads=[c.psN], writes=[c.rstd])
                p.op("act", lambda e: e.activation(c.rstd[:, :], c.rstd[:, :], AF.Sqrt),
                     reads=[c.rstd], writes=[c.rstd])
                p.op("dve", lambda e: e.reciprocal(c.rstd[:, :], c.rstd[:, :]),
                     reads=[c.rstd], writes=[c.rstd])
                for k in range(16):
                    sf = c.stgf[k % 2]
                    p.op("dve", lambda e, k=k, ts=ts, sf=sf: e.scalar_tensor_tensor(
                        sf[:, :], c.xs[:, k, ts], c.gains[:, 16 + k:17 + k], c.rstd[:, :], ALU.mult, ALU.mult),
                        reads=[c.xs, c.rstd, c.gains], writes=[sf])
                    p.dma("sp", yo.t[k * 128:(k + 1) * 128, tok0 + tc * TC:tok0 + (tc + 1) * TC], sf[:, :],
                          reads=[sf], writes=[(yo, (blk, k, tc))], chan=sf.name)
    st = p.emit()
    p.close()
    return nc, st


S = 4096
NEG = -30000.0


class ACtx:
    def __init__(self, p):
        self.p = p
        self.psS = [p.psum("psS%d" % i, [128, 512], F32) for i in range(2)]
        self.psO = [p.psum("psO%d" % i, [128, 512], F32) for i in range(2)]
        self.psL = [p.psum("psL%d" % i, [128, 512], F32) for i in range(2)]
        self.psG = p.psum("psG", [128, 512], F32)
        self.pb = [p.sbuf("pb%d" % i, [128, 512], BF16) for i in range(3)]
        self.ones = p.sbuf("ones", [128, 128], BF16)
        self.rl = p.sbuf("rl", [128, 512], F32)
        self.ob = [p.sbuf("ob%d" % i, [128, 512], BF16) for i in range(2)]
        self.nS = 0
        self.nO = 0
        self.npb = 0
        self.nob = 0
        p.op("dve", lambda e: e.memset(self.ones[:, :], 1.0), writes=[self.ones])


def dense_causal_head(c, qs, ks, vs, out_dram, row0, rope=None, aux=None, nchunks=S // 512):
    p = c.p

    def chunk(ch):
        cs = slice(ch * 512, (ch + 1) * 512)
        nkt = 4 * ch + 4
        O = c.psO[c.nO % 2]
        L = c.psL[c.nO % 2]
        c.nO += 1

        def qk(kt):
            Sb = c.psS[c.nS % 2]
            c.nS += 1
            ksl = slice(kt * 128, (kt + 1) * 128)
            last = (rope is None and aux is None)
            p.op("pe", lambda e: e.matmul(Sb[:, :], ks[:, ksl], qs[:, cs], start=True, stop=last),
                 reads=[ks, qs], writes=[Sb])
            if rope is not None:
                qrs, krs, po = rope
                p.op("pe", lambda e: e.matmul(Sb[:, :], krs[po:po + 64, ksl], qrs[po:po + 64, cs], start=False, stop=(aux is None)),
                     reads=[krs, qrs], writes=[Sb])
            if aux is not None:
                QA, KA, nr = aux
                p.op("pe", lambda e: e.matmul(Sb[:, :], KA[0:nr, ksl], QA[0:nr, cs], start=False, stop=True),
                     reads=[KA, QA], writes=[Sb])
            pb = c.pb[c.npb % 3]
            c.npb += 1
            p.op("act", lambda e: e.activation(pb[:, :], Sb[:, :], AF.Exp), reads=[Sb], writes=[pb])
            if kt >= 4 * ch:
                j = kt - 4 * ch
                p.op("pool", lambda e: e.affine_select(pb[:, :], pb[:, :], [[1, 512]], ALU.is_ge, 0.0,
                                                       base=-128 * j, channel_multiplier=-1),
                     reads=[pb], writes=[pb])
            return pb

        def pv(kt, pb):
            p.op("pe", lambda e: e.matmul(O[:, :], vs[:, kt, :], pb[:, :], start=(kt == 0), stop=(kt == nkt - 1)),
                 reads=[vs, pb], writes=[O])
            p.op("pe", lambda e: e.matmul(L[:, :], c.ones[:, :], pb[:, :], start=(kt == 0), stop=(kt == nkt - 1)),
                 reads=[c.ones, pb], writes=[L])

        cur = qk(0)
        for kt in range(nkt):
            nxt = qk(kt + 1) if kt + 1 < nkt else None
            pv(kt, cur)
            cur = nxt
        ob = c.ob[c.nob % 2]
        c.nob += 1
        p.op("dve", lambda e: e.reciprocal(c.rl[:, :], L[:, :]), reads=[L], writes=[c.rl])
        p.op("dve", lambda e: e.tensor_tensor(ob[:, :], O[:, :], c.rl[:, :], ALU.mult), reads=[O, c.rl], writes=[ob])
        p.dma("sp", out_dram.t[row0:row0 + 128, cs], ob[:, :], reads=[ob], writes=[(out_dram, (row0, ch))], chan=ob.name)

    for ch in range(nchunks):
        chunk(ch)


def moba_gating(c, qs, ks, QA, g):
    p = c.p
    p.op("dve", lambda e: e.tensor_reduce(g.kmf[:, :], ks[:, :].rearrange("p (n j) -> p n j", j=256), AX.X, ALU.add),
         reads=[ks], writes=[g.kmf])
    p.op("act", lambda e: e.activation(g.kmf[:, :], g.kmf[:, :], AF.Copy, scale=1.0 / 256), reads=[g.kmf], writes=[g.kmf])
    p.op("dve", lambda e: e.tensor_copy(g.kmh[:, :], g.kmf[:, :]), reads=[g.kmf], writes=[g.kmh])
    p.op("dve", lambda e: e.tensor_tensor(g.kml[:, :], g.kmf[:, :], g.kmh[:, :], ALU.subtract), reads=[g.kmf, g.kmh], writes=[g.kml])
    p.op("dve", lambda e: e.memset(g.g16[:, :], -1.0e30), writes=[g.g16])
    def tile(qt):
        own = qt // 2
        qsl = slice(qt * 128, (qt + 1) * 128)
        if own > 0:
            p.op("pe", lambda e: e.matmul(c.psG[:, 0:16], qs[:, qsl], g.kmh[:, :], start=True, stop=False),
                 reads=[qs, g.kmh], writes=[c.psG])
            p.op("pe", lambda e: e.matmul(c.psG[:, 0:16], qs[:, qsl], g.kml[:, :], start=False, stop=True),
                 reads=[qs, g.kml], writes=[c.psG])
            p.op("dve", lambda e: e.tensor_copy(g.g16[:, 0:own], c.psG[:, 0:own]), reads=[c.psG], writes=[g.g16])
        p.op("dve", lambda e: e.max(g.m8[:, :], g.g16[:, :]), reads=[g.g16], writes=[g.m8])
        p.op("dve", lambda e: e.tensor_scalar(g.mb[:, :], g.g16[:, :], g.m8[:, 2:3], NEG, ALU.is_lt, ALU.mult),
             reads=[g.g16, g.m8], writes=[g.mb])
        p.op("dve", lambda e: e.memset(g.mb[:, own:own + 1], 0.0), reads=[g.mb], writes=[g.mb])
        p.op("pe", lambda e: e.transpose(c.psG[0:16, 128:256], g.mb[:, :], g.ident[:, :]),
             reads=[g.mb, g.ident], writes=[c.psG])
        p.op("act", lambda e: e.activation(QA[0:16, qsl], c.psG[0:16, 128:256], AF.Copy),
             reads=[c.psG], writes=[(QA, ("m", qt))])

    for qt in range(S // 128):
        tile(qt)


class GBufs:
    def __init__(self, p):
        self.kmf = p.sbuf("kmf", [128, 16], F32)
        self.kmh = p.sbuf("kmh", [128, 16], BF16)
        self.kml = p.sbuf("kml", [128, 16], BF16)
        self.g16 = p.sbuf("g16", [128, 16], F32)
        self.m8 = p.sbuf("m8", [128, 8], F32)
        self.mb = p.sbuf("mb", [128, 16], F32)
        self.ident = p.sbuf("ident_sb", [128, 128], F32)


def build_A_even(nh=4, nchunks=S // 512):
    nc = bass.Bass("TRN2", target_bir_lowering=False)
    p = Prog(nc)
    c = ACtx(p)
    g = GBufs(p)
    di = lambda n, sh, dt=BF16: p.dram(n, sh, dt, kind="ExternalInput")
    qn = di("qn", [nh * 128, S]); qr = di("qr", [nh * 64, S]); kn = di("kn", [nh * 128, S]); kr2 = di("kr2", [128, S])
    mv = di("mv", [S, nh * 128])
    bq = di("bq", [nh * 128, S]); bk = di("bk", [nh * 128, S]); bv = di("bv", [S, nh * 128])
    KAd = di("KA", [36, S]); QAd = di("QAc", [nh, 4, S]); identd = di("ident", [128, 128], F32)
    atT = p.dram("atT", [2 * nh * 128, S], BF16, kind="ExternalOutput")
    qs = [p.sbuf("qs%d" % i, [128, S], BF16) for i in range(2)]
    ks = [p.sbuf("ks%d" % i, [128, S], BF16) for i in range(2)]
    vs = [p.sbuf("vs%d" % i, [128, S // 128, 128], BF16) for i in range(2)]
    qrs = p.sbuf("qrs", [128, S], BF16)
    krs = p.sbuf("krs", [128, S], BF16)
    KA = p.sbuf("KAs", [36, S], BF16)
    QA = [p.sbuf("QA%d" % i, [36, S], BF16) for i in range(2)]
    p.dma("sp", krs[:, :], kr2[:, :], reads=[kr2], writes=[krs])
    p.dma("sp", KA[:, :], KAd[:, :], reads=[KAd], writes=[KA])
    p.dma("sp", g.ident[:, :], identd[:, :], reads=[identd], writes=[g.ident])
    nb = 0
    for h in range(nh):
        q_, k_, v_ = qs[nb % 2], ks[nb % 2], vs[nb % 2]
        nb += 1
        p.dma("sp", q_[:, :], qn.t[h * 128:(h + 1) * 128, :], reads=[qn], writes=[q_])
        p.dma("sp", k_[:, :], kn.t[h * 128:(h + 1) * 128, :], reads=[kn], writes=[k_])
        p.dma("sp", v_[:, :, :], mv.t[:, h * 128:(h + 1) * 128].rearrange("(t p) d -> p t d", p=128), reads=[mv], writes=[v_])
        if h % 2 == 0:
            p.dma("sp", qrs[:, :], qr.t[(h // 2) * 128:(h // 2 + 1) * 128, :], reads=[qr], writes=[qrs])
        dense_causal_head(c, q_, k_, v_, atT, h * 128, rope=(qrs, krs, (h % 2) * 64), nchunks=nchunks)
    for h in range(nh):
        q_, k_, v_ = qs[nb % 2], ks[nb % 2], vs[nb % 2]
        QA_ = QA[nb % 2]
        nb += 1
        p.dma("sp", q_[:, :], bq.t[h * 128:(h + 1) * 128, :], reads=[bq], writes=[q_])
        p.dma("sp", k_[:, :], bk.t[h * 128:(h + 1) * 128, :], reads=[bk], writes=[k_])
        p.dma("sp", v_[:, :, :], bv.t[:, h * 128:(h + 1) * 128].rearrange("(t p) d -> p t d", p=128), reads=[bv], writes=[v_])
        p.op("dve", lambda e, QA_=QA_: e.memset(QA_[:, :], 0.0), writes=[QA_])
        p.dma("sp", QA_[32:36, :], QAd.t[h, :, :], reads=[QAd], writes=[(QA_, "al")])
        moba_gating(c, q_, k_, QA_, g)
        dense_causal_head(c, q_, k_, v_, atT, (nh + h) * 128, aux=(QA_, KA, 36), nchunks=nchunks)
    st = p.emit()
    p.close()
    return nc, st


DIL_D = (1, 4, 16)


def build_A_odd(ngroups=3, nslots=2, nswa=8):
    nc = bass.Bass("TRN2", target_bir_lowering=False)
    p = Prog(nc)
    c = ACtx(p)
    di = lambda n, sh, dt=BF16: p.dram(n, sh, dt, kind="ExternalInput")
    dq = di("dq", [6 * 128, S]); dk = di("dk", [6 * 128, S]); dv = di("dv", [S, 6 * 128])
    sq = di("sq", [512, S]); sk2 = di("sk2", [128, S]); sv = di("sv", [S, 64])
    KAwd = di("KAw", [4, 14 * 128]); QAwd = di("QAw", [4, 14 * 2 * 128]); maskd = di("masks", [128, 3 * 128]); identd = di("identb", [128, 128])
    esd = di("sinks", [128, 8], F32)
    atT = p.dram("atT", [768, S], BF16, kind="ExternalOutput")
    qs = [p.sbuf("qs%d" % i, [128, S], BF16) for i in range(2)]
    ks = [p.sbuf("ks%d" % i, [128, S], BF16) for i in range(2)]
    vs = [p.sbuf("vs%d" % i, [128, S // 128, 128], BF16) for i in range(2)]
    Oacc = p.sbuf("Oacc", [128, S], F32)
    Lacc = p.sbuf("Lacc", [128, S], F32)
    KAw = p.sbuf("KAws", [4, 14 * 128], BF16)
    QAw = p.sbuf("QAws", [4, 14 * 2 * 128], BF16)
    masks = p.sbuf("maskss", [128, 3 * 128], BF16)
    identb = p.sbuf("identbs", [128, 128], BF16)
    es = p.sbuf("es", [128, 8], F32)
    for (sb, dd) in ((KAw, KAwd), (QAw, QAwd), (masks, maskd), (identb, identd), (es, esd)):
        p.dma("sp", sb[:, :], dd[:, :], reads=[dd], writes=[sb])
    p.op("act", lambda e: e.activation(es[:, :], es[:, :], AF.Exp), reads=[es], writes=[es])

    def win_tile(q_ap, k_aps, v_aps, hw, prev_mask, O_ap, L_ap, O, L):
        items = [(w, k_aps[w], v_aps[w]) for w in (0, 1) if k_aps[w] is not None]
        for n, (w, k_ap, v_ap) in enumerate(items):
            Sb = c.psS[c.nS % 2]
            c.nS += 1
            mi = 0 if w == 1 else prev_mask
            p.op("pe", lambda e, Sb=Sb, k_ap=k_ap: e.matmul(Sb[:, 0:128], k_ap, q_ap, start=True, stop=False),
                 reads=[c.kq_bufs[0], c.kq_bufs[1]], writes=[Sb])
            p.op("pe", lambda e, Sb=Sb, w=w: e.matmul(Sb[:, 0:128], KAw[0:4, hw * 128:(hw + 1) * 128],
                                                     QAw[0:4, (hw * 2 + w) * 128:(hw * 2 + w + 1) * 128], start=False, stop=False),
                 reads=[KAw, QAw], writes=[Sb])
            p.op("pe", lambda e, Sb=Sb, mi=mi: e.matmul(Sb[:, 0:128], identb[:, :], masks[:, mi * 128:(mi + 1) * 128], start=False, stop=True),
                 reads=[identb, masks], writes=[Sb])
            pb = c.pb[c.npb % 3]
            c.npb += 1
            p.op("act", lambda e, Sb=Sb, pb=pb: e.activation(pb[:, 0:128], Sb[:, 0:128], AF.Exp), reads=[Sb], writes=[pb])
            first, last = (n == 0), (n == len(items) - 1)
            p.op("pe", lambda e, pb=pb, v_ap=v_ap, first=first, last=last: e.matmul(O_ap, v_ap, pb[:, 0:128], start=first, stop=last),
                 reads=[c.kq_bufs[2], pb], writes=[O])
            p.op("pe", lambda e, pb=pb, first=first, last=last: e.matmul(L_ap, c.ones[:, :], pb[:, 0:128], start=first, stop=last),
                 reads=[c.ones, pb], writes=[L])

    nb = 0
    for jl in range(nslots):
        for g in range(ngroups):
            d = DIL_D[g]
            hw = jl * 3 + g
            q_, k_, v_ = qs[nb % 2], ks[nb % 2], vs[nb % 2]
            nb += 1
            c.kq_bufs = (k_, q_, v_)
            p.dma("sp", q_[:, :], dq.t[hw * 128:(hw + 1) * 128, :], reads=[dq], writes=[q_])
            p.dma("sp", k_[:, :], dk.t[hw * 128:(hw + 1) * 128, :], reads=[dk], writes=[k_])
            nbk = S // (128 * d)
            for r in range(d):
                src = dv.t[:, hw * 128:(hw + 1) * 128].rearrange("(b p r) e -> r p b e", p=128, r=d)[r]
                p.dma("sp", v_[:, r * nbk:(r + 1) * nbk, :], src, reads=[dv], writes=[(v_, r)], chan=v_.name + "_%d" % (r % 4))
            bat = min(4, nbk)
            for r in range(d):
                for b0 in range(0, nbk, bat):
                    O = c.psO[c.nO % 2]
                    L = c.psL[c.nO % 2]
                    c.nO += 1
                    for jb in range(bat):
                        b = b0 + jb
                        col = lambda bb: slice(r + d * 128 * bb, r + d * 128 * bb + d * 127 + 1, d)
                        q_ap = q_[:, col(b)]
                        k_aps = [k_[:, col(b - 1)] if b > 0 else None, k_[:, col(b)]]
                        v_aps = [v_[:, r * nbk + b - 1, :] if b > 0 else None, v_[:, r * nbk + b, :]]
                        win_tile(q_ap, k_aps, v_aps, hw, 1, O[:, jb * 128:(jb + 1) * 128], L[:, jb * 128:(jb + 1) * 128], O, L)
                    n = bat * 128
                    tsl = slice(r + d * 128 * b0, r + d * 128 * b0 + d * (bat * 128 - 1) + 1, d)
                    if g == 0:
                        p.op("act", lambda e, O=O, tsl=tsl, n=n: e.activation(Oacc[:, tsl], O[:, 0:n], AF.Copy), reads=[O], writes=[Oacc])
                        p.op("act", lambda e, L=L, tsl=tsl, n=n: e.activation(Lacc[:, tsl], L[:, 0:n], AF.Copy), reads=[L], writes=[Lacc])
                    else:
                        p.op("dve", lambda e, O=O, tsl=tsl, n=n: e.tensor_tensor(Oacc[:, tsl], O[:, 0:n], Oacc[:, tsl], ALU.add), reads=[O, Oacc], writes=[Oacc])
                        p.op("dve", lambda e, L=L, tsl=tsl, n=n: e.tensor_tensor(Lacc[:, tsl], L[:, 0:n], Lacc[:, tsl], ALU.add), reads=[L, Lacc], writes=[Lacc])
        for ch in range(S // 512):
            cs = slice(ch * 512, (ch + 1) * 512)
            ob = c.ob[c.nob % 2]
            c.nob += 1
            p.op("dve", lambda e, cs=cs: e.reciprocal(c.rl[:, :], Lacc[:, cs]), reads=[Lacc], writes=[c.rl])
            p.op("dve", lambda e, cs=cs, ob=ob: e.tensor_tensor(ob[:, :], Oacc[:, cs], c.rl[:, :], ALU.mult), reads=[Oacc, c.rl], writes=[ob])
            p.dma("sp", atT.t[jl * 128:(jl + 1) * 128, cs], ob[:, :], reads=[ob], writes=[(atT, (jl, ch))], chan=ob.name)
    if nswa:
        k_ = ks[nb % 2]
        v_ = vs[nb % 2]
        p.dma("sp", k_[:, :], sk2[:, :], reads=[sk2], writes=[k_])
        p.dma("sp", v_[:, :, 0:64], sv.t[:, :].rearrange("(t p) e -> p t e", p=128), reads=[sv], writes=[(v_, 0)], chan=v_.name + "_0")
        p.dma("sp", v_[:, :, 64:128], sv.t[:, :].rearrange("(t p) e -> p t e", p=128), reads=[sv], writes=[(v_, 1)], chan=v_.name + "_1")
        for hl in range(nswa):
            if hl % 2 == 0:
                q_ = qs[(hl // 2) % 2]
                p.dma("sp", q_[:, :], sq.t[(hl // 2) * 128:(hl // 2 + 1) * 128, :], reads=[sq], writes=[q_])
            c.kq_bufs = (k_, q_, v_)
            po = (hl % 2) * 64
            hw = 6 + hl
            for b0 in range(0, S // 128, 4):
                O = c.psO[c.nO % 2]
                L = c.psL[c.nO % 2]
                c.nO += 1
                for jb in range(4):
                    b = b0 + jb
                    col = lambda bb: slice(128 * bb, 128 * (bb + 1))
                    q_ap = q_[po:po + 64, col(b)]
                    k_aps = [k_[po:po + 64, col(b - 1)] if b > 0 else None, k_[po:po + 64, col(b)]]
                    v_aps = [v_[:, b - 1, :] if b > 0 else None, v_[:, b, :]]
                    win_tile(q_ap, k_aps, v_aps, hw, 2, O[:, jb * 128:(jb + 1) * 128], L[:, jb * 128:(jb + 1) * 128], O, L)
                cs = slice(b0 * 128, b0 * 128 + 512)
                ob = c.ob[c.nob % 2]
                c.nob += 1
                p.op("dve", lambda e, L=L, hl=hl, po=po: e.tensor_scalar(c.rl[po:po + 64, :], L[po:po + 64, :], es[po:po + 64, hl:hl + 1], None, ALU.add),
                     reads=[L, es], writes=[c.rl])
                p.op("dve", lambda e, po=po: e.reciprocal(c.rl[po:po + 64, :], c.rl[po:po + 64, :]), reads=[c.rl], writes=[c.rl])
                p.op("dve", lambda e, O=O, ob=ob, po=po: e.tensor_tensor(ob[po:po + 64, :], O[po:po + 64, :], c.rl[po:po + 64, :], ALU.mult),
                     reads=[O, c.rl], writes=[ob])
                p.dma("sp", atT.t[256 + hl * 64:256 + (hl + 1) * 64, cs], ob[po:po + 64, :], reads=[ob], writes=[(atT, ("s", hl, b0))], chan=ob.name)
    st = p.emit()
    p.close()
    return nc, st

import numpy as np
import ml_dtypes

def gain_layout(g):
    return np.ascontiguousarray(np.asarray(g, np.float32).reshape(-1, 128).T)

def prep_w_in_odd(w):
    cd = 1536
    dq, dk, dv = w[:, :cd], w[:, cd:2 * cd], w[:, 2 * cd:3 * cd]
    o = 3 * cd
    sq = w[:, o:o + 1024]
    sk = w[:, o + 1024:o + 1152]
    sv = w[:, o + 1152:o + 1280]
    sk2 = np.concatenate([sk[:, :64], sk[:, :64], sk[:, 64:], sk[:, 64:]], axis=1)
    return np.ascontiguousarray(np.concatenate([dq, dk, dv, sq, sk2, sv], axis=1))

def _swap(a):
    n = a.shape[1] // 64
    b = a.reshape(a.shape[0], n, 2, 32)[:, :, ::-1, :]
    return b.reshape(a.shape[0], n * 64)

def prep_w_in_even(w):
    qlat, kvlat, kpe = w[:, :512], w[:, 512:1024], w[:, 1024:1088]
    rest = w[:, 1088:]
    krx = np.concatenate([kpe, kpe], axis=1)
    krs = _swap(krx)
    return np.ascontiguousarray(np.concatenate([qlat, kvlat, krx, krs, rest], axis=1))

def prep_w_uq(w):
    w3 = w.reshape(512, 8, 192)
    nope = w3[:, :, :128].reshape(512, 1024)
    rope = w3[:, :, 128:].reshape(512, 512)
    rsw = _swap(rope)
    parts = [nope]
    for j in range(4):
        parts += [rope[:, j * 128:(j + 1) * 128], rsw[:, j * 128:(j + 1) * 128]]
    return np.ascontiguousarray(np.concatenate(parts, axis=1))

def prep_w_ukv(w):
    w3 = w.reshape(512, 8, 256)
    return np.ascontiguousarray(np.concatenate([w3[:, :, :128].reshape(512, 1024), w3[:, :, 128:].reshape(512, 1024)], axis=1))

def rope_tables(S=4096):
    half = 32
    inv = (10000.0 ** (-np.arange(half, dtype=np.float32) / half)).astype(np.float32)
    ang = np.arange(S, dtype=np.float32)[None, :] * inv[:, None]
    cos, sin = np.cos(ang).astype(np.float32), np.sin(ang).astype(np.float32)
    C = np.concatenate([cos, cos, cos, cos], axis=0)
    Sg = np.concatenate([-sin, sin, -sin, sin], axis=0)
    return np.ascontiguousarray(C), np.ascontiguousarray(Sg)

def make_KA(S=4096):
    KA = np.zeros((36, S), np.float32)
    s = np.arange(S)
    KA[s // 256, s] = 1.0
    KA[32] = 1.0
    KA[33] = 1.0
    KA[34] = 128.0 * (s // 128)
    KA[35] = s % 128
    return KA.astype(ml_dtypes.bfloat16)

def make_QAc(heads, nheads_total=8, S=4096):
    t = np.arange(S)
    out = np.zeros((len(heads), 4, S), np.float32)
    for i, h in enumerate(heads):
        slope = 2.0 ** (-8.0 * (h + 1) / nheads_total)
        out[i, 0] = -slope * 128.0 * (t // 128)
        out[i, 1] = -slope * (t % 128)
        out[i, 2] = slope
        out[i, 3] = slope
    return out.astype(ml_dtypes.bfloat16)

def _hilo(x):
    hi = np.float32(x).astype(ml_dtypes.bfloat16)
    lo = (np.float32(x) - hi.astype(np.float32)).astype(ml_dtypes.bfloat16)
    return hi, lo

def make_win_consts(hh):
    cs = []
    for jl in range(2):
        for g in range(3):
            hd = g * 4 + (2 * hh + jl)
            slope = 2.0 ** (-8.0 * (hd + 1) / 12)
            cs.append(slope * (1, 4, 16)[g])
    for hl in range(8):
        hq = 8 * hh + hl
        cs.append(2.0 ** (-8.0 * (hq + 1) / 16))
    KAw = np.zeros((4, 14, 128), np.float32)
    QAw = np.zeros((4, 14, 2, 128), np.float32)
    sl = np.arange(128, dtype=np.float32)
    for i, cval in enumerate(cs):
        hi, lo = _hilo(cval)
        hi, lo = np.float32(hi), np.float32(lo)
        KAw[0, i] = hi; KAw[1, i] = lo; KAw[2, i] = sl; KAw[3, i] = sl
        for w, off in ((0, 128.0), (1, 0.0)):
            QAw[0, i, w] = -(sl + off); QAw[1, i, w] = -(sl + off); QAw[2, i, w] = hi; QAw[3, i, w] = lo
    s_ = np.arange(128)[:, None]; t_ = np.arange(128)[None, :]
    NEGV = -30000.0
    m_own = np.where(t_ >= s_, 0.0, NEGV)
    m_p128 = np.where(t_ <= s_, 0.0, NEGV)
    m_p127 = np.where(t_ < s_, 0.0, NEGV)
    masks = np.concatenate([m_own, m_p128, m_p127], axis=1)
    bf = ml_dtypes.bfloat16
    return (KAw.reshape(4, -1).astype(bf), QAw.reshape(4, -1).astype(bf), masks.astype(bf), np.eye(128, dtype=np.float32).astype(bf))


_PROGS = {}


def _prog(key, fn):
    if key not in _PROGS:
        _PROGS[key] = fn()[0]
    return _PROGS[key]


def kernel(x, attn_norm, mlp_norm, w_up, w_down, ev_w_in, ev_q_norm, ev_w_uq, ev_kv_norm,
           ev_w_ukv, ev_w_out, od_w_in, od_sinks, od_w_out, final_norm):
    from concourse.bass_utils import run_bass_kernel_spmd
    A = lambda a: np.ascontiguousarray(np.asarray(a))
    x = A(x)
    B, SS, DD = x.shape
    ncore = 8
    cores = list(range(ncore))
    xT = [A(x[c // 2, (c % 2) * 2048:(c % 2 + 1) * 2048, :].T) for c in cores]
    C_, S_ = rope_tables()
    cosd = [A(C_[:, (c % 2) * 2048:(c % 2 + 1) * 2048]) for c in cores]
    sind = [A(S_[:, (c % 2) * 2048:(c % 2 + 1) * 2048]) for c in cores]
    KA = make_KA()
    ident = np.eye(128, dtype=np.float32)
    atT = None
    for i in range(5):
        nc = _prog(("L", i if i in (0, 4) else 1 + (i % 2 == 0)), lambda i=i: build_L(i))
        common = {}
        if i > 0:
            l = i - 1
            wo = A(ev_w_out[l // 2]) if l % 2 == 0 else A(od_w_out[l // 2])
            common.update(w_out=wo, g_mlp=gain_layout(mlp_norm[l]), w_up=A(w_up[l]), w_dn=A(w_down[l]))
        if i < 4:
            common.update(g_att=gain_layout(attn_norm[i]))
            if i % 2 == 0:
                common.update(w_in=prep_w_in_even(A(ev_w_in[i // 2])), g_q=gain_layout(ev_q_norm[i // 2]), g_kv=gain_layout(ev_kv_norm[i // 2]),
                              w_uq=prep_w_uq(A(ev_w_uq[i // 2])), w_ukv=prep_w_ukv(A(ev_w_ukv[i // 2])))
            else:
                common.update(w_in=prep_w_in_odd(A(od_w_in[i // 2])))
        else:
            common.update(g_fin=gain_layout(final_norm))
        in_maps = []
        for c in cores:
            m = dict(common)
            m["xT"] = xT[c]
            if i > 0:
                m["atT"] = atT[c]
            if i < 4 and i % 2 == 0:
                m["cosd"] = cosd[c]
                m["sind"] = sind[c]
            in_maps.append(m)
        res = run_bass_kernel_spmd(nc, in_maps, core_ids=cores).results
        if i == 4:
            out = np.empty((B, SS, DD), np.float32)
            for c in cores:
                out[c // 2, (c % 2) * 2048:(c % 2 + 1) * 2048, :] = res[c]["yo"].T
            return out
        xT = [res[c]["xo"] for c in cores]
        full = {}
        names = list(OUT_SHAPES_EVEN if i % 2 == 0 else OUT_SHAPES_ODD)
        shapes = OUT_SHAPES_EVEN if i % 2 == 0 else OUT_SHAPES_ODD
        for b in range(B):
            for nm in names:
                ax = 1 if shapes[nm][0] == "fm" else 0
                full[(b, nm)] = np.concatenate([res[2 * b]["o_" + nm], res[2 * b + 1]["o_" + nm]], axis=ax)
        in_maps = []
        if i % 2 == 0:
            nc = _prog(("A", 0), build_A_even)
            for c in cores:
                b, hh = c // 2, c % 2
                f = lambda nm: full[(b, nm)]
                in_maps.append(dict(
                    qn=A(f("qn")[hh * 512:(hh + 1) * 512]), qr=A(f("qr")[hh * 256:(hh + 1) * 256]), kn=A(f("kn")[hh * 512:(hh + 1) * 512]),
                    kr2=A(f("kr2")), mv=A(f("mv")[:, hh * 512:(hh + 1) * 512]),
                    bq=A(f("bq")[hh * 512:(hh + 1) * 512]), bk=A(f("bk")[hh * 512:(hh + 1) * 512]), bv=A(f("bv")[:, hh * 512:(hh + 1) * 512]),
                    KA=KA, QAc=make_QAc(list(range(4 * hh, 4 * hh + 4))), ident=ident))
            res = run_bass_kernel_spmd(nc, in_maps, core_ids=cores).results
            atT = []
            for c in cores:
                b, half = c // 2, c % 2
                ts = slice(half * 2048, (half + 1) * 2048)
                r0, r1 = res[2 * b]["atT"], res[2 * b + 1]["atT"]
                atT.append(A(np.concatenate([r0[0:512, ts], r1[0:512, ts], r0[512:1024, ts], r1[512:1024, ts]], axis=0)))
        else:
            nc = _prog(("A", 1), build_A_odd)
            sinks = np.asarray(od_sinks[i // 2], np.float32)
            for c in cores:
                b, hh = c // 2, c % 2
                f = lambda nm: full[(b, nm)]
                hds = [g * 4 + (2 * hh + jl) for jl in range(2) for g in range(3)]
                rows = np.concatenate([np.arange(h * 128, (h + 1) * 128) for h in hds])
                KAw, QAw, masks, identb = make_win_consts(hh)
                in_maps.append(dict(
                    dq=A(f("dq")[rows]), dk=A(f("dk")[rows]), dv=A(f("dv")[:, rows]),
                    sq=A(f("sq")[hh * 512:(hh + 1) * 512]), sk2=A(f("sk2")[hh * 128:(hh + 1) * 128]), sv=A(f("sv")[:, hh * 64:(hh + 1) * 64]),
                    KAw=KAw, QAw=QAw, masks=masks, identb=identb,
                    sinks=A(np.tile(sinks[None, 8 * hh:8 * hh + 8], (128, 1)))))
            res = run_bass_kernel_spmd(nc, in_maps, core_ids=cores).results
            atT = []
            for c in cores:
                b, half = c // 2, c % 2
                ts = slice(half * 2048, (half + 1) * 2048)
                r0, r1 = res[2 * b]["atT"], res[2 * b + 1]["atT"]
                atT.append(A(np.concatenate([r0[0:256, ts], r1[0:256, ts], r0[256:768, ts], r1[256:768, ts]], axis=0)))
```

```python
import numpy as np
import concourse.bass as bass
import concourse.mybir as mybir
from contextlib import ExitStack

F32 = mybir.dt.float32
BF16 = mybir.dt.bfloat16
ALU = mybir.AluOpType
AF = mybir.ActivationFunctionType
AX = mybir.AxisListType

ENGS = ("pe", "act", "dve", "pool", "sp")


class Buf:
    def __init__(self, name, t):
        self.name = name
        self.t = t
        self.st = {}

    def __getitem__(self, idx):
        return self.t[idx]

    def view(self, name, ap):
        v = Buf(name, ap)
        v.st = self.st
        return v


class Op:
    __slots__ = ("eng", "fn", "reads", "writes", "dma", "chan", "deps", "sig", "ordv", "inc")

    def __init__(self, eng, fn, reads, writes, dma=False, chan=None):
        self.eng = eng
        self.fn = fn
        self.reads = reads
        self.writes = writes
        self.dma = dma
        self.chan = chan
        self.deps = ()
        self.sig = False
        self.ordv = 0
        self.inc = 16


def _norm(lst):
    out = []
    for x in lst:
        if isinstance(x, tuple):
            out.append(x)
        else:
            out.append((x, None))
    return out


class Prog:
    def __init__(self, nc, same_engine_raw=True):
        self.nc = nc
        self.ops = []
        self.es = ExitStack()
        self.same_engine_raw = same_engine_raw
        self.engobj = {"pe": nc.tensor, "act": nc.scalar, "dve": nc.vector,
                       "pool": nc.gpsimd, "sp": nc.sync}
        self.nbuf = 0

    def sbuf(self, name, shape, dt):
        t = self.es.enter_context(self.nc.sbuf_tensor(name, list(shape), dt))
        return Buf(name, t)

    def psum(self, name, shape, dt):
        t = self.es.enter_context(self.nc.psum_tensor(name, list(shape), dt))
        return Buf(name, t)

    def dram(self, name, shape, dt, kind="Internal"):
        t = self.nc.dram_tensor(name, list(shape), dt, kind=kind)
        return Buf(name, t.ap())

    def op(self, eng, fn, reads=(), writes=()):
        self.ops.append(Op(eng, fn, _norm(reads), _norm(writes)))

    def barrier(self):
        self.ops.append(Op("bar", None, [], []))

    def coll(self, kind, groups, in_buf, out_buf):
        def fn(e, in_buf=in_buf, out_buf=out_buf):
            return e.collective_compute(kind, ALU.bypass, replica_groups=groups,
                                        ins=[in_buf.t[:, :].opt()], outs=[out_buf.t[:, :].opt()])
        o = Op("pool", fn, [(in_buf, None)], [(out_buf, None)], dma=True, chan="cc")
        o.inc = 1
        self.ops.append(o)

    def arena_init(self, nbytes):
        self.arena = self.es.enter_context(self.nc.sbuf_tensor("arena", [128, nbytes], mybir.dt.uint8))
        self.arena_off = {}

    def sb(self, group, name, shape, dt, base=0):
        esz = 4 if dt == F32 or dt == mybir.dt.int32 else 2
        n = 1
        for d in shape[1:]:
            n *= d
        nb = (n * esz + 31) // 32 * 32
        off = self.arena_off.get(group, base)
        self.arena_off[group] = off + nb
        assert off + nb <= self.arena.shape[1], (group, name, off + nb)
        ap = self.arena[0:shape[0], off:off + n * esz].bitcast(dt)
        if len(shape) == 3:
            ap = ap.rearrange("p (a b) -> p a b", b=shape[2])
        return Buf(name, ap)

    def dma(self, q, out_ap, in_ap, reads=(), writes=(), chan=None, **kw):
        def fn(e, out_ap=out_ap, in_ap=in_ap, kw=kw):
            return e.dma_start(out=out_ap, in_=in_ap, **kw)
        writes = _norm(writes)
        if chan is None:
            chan = "%s:%s" % (writes[0][0].name, writes[0][1])
        self.ops.append(Op(q, fn, _norm(reads), writes, dma=True, chan=chan))

    @staticmethod
    def _states(buf, key):
        st = buf.st
        if key is None:
            if None not in st:
                st[None] = [None, {}]
            return list(st.values())
        res = []
        if None in st:
            res.append(st[None])
        if key not in st:
            st[key] = [None, {}]
        res.append(st[key])
        return res

    def _analyze(self):
        last = {}
        bar = set()
        for i, o in enumerate(self.ops):
            if o.eng == "bar":
                bar = set(last.values())
                o.deps = ()
                continue
            deps = set(bar)
            ek = ("dma", o.chan) if o.dma else o.eng
            last[ek] = i
            for (b, k) in o.reads:
                for s in self._states(b, k):
                    if s[0] is not None:
                        deps.add(s[0])
            for (b, k) in o.writes:
                for s in self._states(b, k):
                    if s[0] is not None:
                        deps.add(s[0])
                    deps.update(s[1].values())
            deps.discard(i)
            for (b, k) in o.reads:
                if k is None:
                    for s in self._states(b, None):
                        s[1][ek] = i
                else:
                    self._states(b, k)[-1][1][ek] = i
            for (b, k) in o.writes:
                if k is None:
                    b.st.clear()
                    b.st[None] = [i, {}]
                else:
                    s = self._states(b, k)[-1]
                    s[0] = i
                    s[1] = {}
            o.deps = deps

    def emit(self):
        nc = self.nc
        self._analyze()
        ops = self.ops
        for i, o in enumerate(ops):
            if o.eng == "bar":
                continue
            per = {}
            for d in o.deps:
                od = ops[d]
                if od.dma:
                    key = ("dma", od.chan)
                else:
                    if od.eng == o.eng and not o.dma:
                        if od.eng == "pe" or not self.same_engine_raw:
                            continue
                        raw = any((b.st is rb.st and (k is None or rk is None or k == rk))
                                  for (b, k) in od.writes for (rb, rk) in o.reads)
                        if not raw:
                            continue
                    key = ("eng", od.eng)
                if key not in per or per[key] < d:
                    per[key] = d
            o.deps = sorted(per.values())
            for d in o.deps:
                ops[d].sig = True
        cnt = {e: 0 for e in ENGS}
        for o in ops:
            if o.dma or o.eng == "bar":
                continue
            if o.sig:
                cnt[o.eng] += 1
                o.ordv = cnt[o.eng]
        sems = {e: self.es.enter_context(nc.semaphore("s_" + e)) for e in ENGS}
        chans = {}
        chan_total = {}
        known = {e: {} for e in ENGS}
        nwait = 0
        for i, o in enumerate(ops):
            if o.eng == "bar":
                continue
            e = self.engobj[o.eng]
            kn = known[o.eng]
            for d in o.deps:
                od = ops[d]
                if od.dma:
                    key = ("dma", od.chan)
                    val = chan_total[od.chan]
                    sem = chans[od.chan]
                else:
                    key = ("eng", od.eng)
                    val = od.ordv
                    sem = sems[od.eng]
                if kn.get(key, 0) >= val:
                    continue
                e.wait_ge(sem, val)
                nwait += 1
                kn[key] = val
            ins = o.fn(e)
            if o.dma:
                ch = o.chan
                if ch not in chans:
                    chans[ch] = self.es.enter_context(nc.semaphore("d_" + str(ch)))
                    chan_total[ch] = 0
                chan_total[ch] += o.inc
                ins.then_inc(chans[ch], o.inc)
            elif o.sig:
                ins.then_inc(sems[o.eng], 1)
        for ch, tot in chan_total.items():
            self.engobj["sp"].wait_ge(chans[ch], tot)
        self.stats = dict(n_ops=len(ops), n_wait=nwait, n_chan=len(chans),
                          sig={e: cnt[e] for e in ENGS})
        return self.stats

    def close(self):
        self.es.close()


D = 2048
DFF = 8192
TB = 1024
TC = 512
EPS = 1e-6
NEG = -30000.0


class LCtx:
    def __init__(self, p, ps, ones, gains):
        self.p = p
        sb = lambda n, sh, dt: p.sb("L", n, sh, dt, base=4096)
        self.xs = sb("xs", [128, 16, TB], F32)
        self.hT = sb("hT", [128, 16, TB], BF16)
        self.wA = [sb("wA%d" % i, [128, 16, 512], BF16) for i in range(2)]
        self.wB = [sb("wB%d" % i, [128, 4, 2048], BF16) for i in range(2)]
        self.uT = sb("uT", [128, 4, TB], BF16)
        self.sq = [sb("sq%d" % i, [128, 512], BF16) for i in range(2)]
        self.rstd = sb("rstd", [128, 512], F32)
        self.ones = ones
        self.stg = [sb("stg%d" % i, [128, 1024], BF16) for i in range(2)]
        self.stgf = [sb("stgf%d" % i, [128, 512], F32) for i in range(2)]
        self.gains = gains
        self.ps = ps[0:4]
        self.psN = ps[4]
        self.psY = ps[5:7]
        self.nps = 0
        self.nY = 0
        self.nw = 0
        self.nsq = 0
        self.nstg = 0
        self.nalt = 0
        self.lat = self.wB[0].view("lat", self.wB[0].t[:, :, :].bitcast(F32))
        self.candB = self.wB[1].view("candB", self.wB[1].t[:, :, :].rearrange("p a (b c) -> p (a b) c", c=TB))
        self.latn = sb("latn", [128, 4, TB], BF16)
        self.ropeb = sb("ropeb", [128, 1, TB], F32)
        self.cosT = sb("cosT", [128, TB], F32)
        self.sinT = sb("sinT", [128, TB], F32)

    def next_ps(self):
        b = self.ps[self.nps % 4]
        self.nps += 1
        return b

    def next_psY(self):
        b = self.psY[self.nY % 2]
        self.nY += 1
        return b

    def next_stg(self):
        b = self.stg[self.nstg % 2]
        self.nstg += 1
        return b


def load_gain(c, g_dram, col0, nk):
    p = c.p
    p.dma("sp", c.gains[:, col0:col0 + nk], g_dram[:, :], reads=[g_dram], writes=[(c.gains, col0)],
          chan="gains%d" % col0)


def rmsnorm_fm(c, src, nk, gcol, dst, dfeat, ntc, src_key=None):
    p = c.p
    for tc in range(ntc):
        ts = slice(tc * TC, (tc + 1) * TC)
        for k in range(nk):
            sq = c.sq[c.nsq % 2]
            c.nsq += 1
            p.op("act", lambda e, sq=sq, k=k, ts=ts: e.activation(sq[:, :], src[:, k, ts], AF.Square),
                 reads=[src], writes=[sq])
            p.op("pe", lambda e, sq=sq, k=k: e.matmul(c.psN[:, :], c.ones[:, :], sq[:, :], start=(k == 0), stop=(k == nk - 1)),
                 reads=[sq, c.ones], writes=[c.psN])
        p.op("dve", lambda e: e.tensor_scalar(c.rstd[:, :], c.psN[:, :], 1.0 / dfeat, EPS, ALU.mult, ALU.add),
             reads=[c.psN], writes=[c.rstd])
        p.op("act", lambda e: e.activation(c.rstd[:, :], c.rstd[:, :], AF.Sqrt),
             reads=[c.rstd], writes=[c.rstd])
        p.op("dve", lambda e: e.reciprocal(c.rstd[:, :], c.rstd[:, :]),
             reads=[c.rstd], writes=[c.rstd])
        for k in range(nk):
            p.op("dve", lambda e, k=k, ts=ts: e.scalar_tensor_tensor(
                dst[:, k, ts], src[:, k, ts], c.gains[:, gcol + k:gcol + k + 1], c.rstd[:, :], ALU.mult, ALU.mult),
                reads=[src, c.rstd, c.gains], writes=[(dst, ("n", k, tc))])


def load_w_slab(c, wbuf, w_dram, r0, nk, c0, ncols):
    p = c.p
    src = w_dram.t[r0:r0 + nk * 128, c0:c0 + ncols].rearrange("(k p) n -> p k n", p=128)
    p.dma("pool", wbuf[:, 0:nk, 0:ncols], src, reads=[w_dram], writes=[wbuf], chan=wbuf.name)


def lin_fm(c, act, nk, ntc, w_dram, r0, c0, ncols, evac, act_keys=None):
    p = c.p
    done = 0
    while done < ncols:
        n = min(512, ncols - done)
        wb = c.wA[c.nw % 2]
        c.nw += 1
        load_w_slab(c, wb, w_dram, r0, nk, c0 + done, n)
        for jj in range(n // 128):
            j = (done // 128) + jj
            for tc in range(ntc):
                ps = c.next_ps()
                for k in range(nk):
                    p.op("pe", lambda e, ps=ps, wb=wb, k=k, jj=jj, tc=tc: e.matmul(
                        ps[:, :], wb[:, k, jj * 128:(jj + 1) * 128], act[:, k, tc * TC:(tc + 1) * TC],
                        start=(k == 0), stop=(k == nk - 1)),
                        reads=[wb, act], writes=[ps])
                evac(j, tc, ps)
        done += n


def lin_tm(c, act, nk, ntt, w_dram, r0, c0, ncols, evac):
    p = c.p
    done = 0
    while done < ncols:
        n = min(512, ncols - done)
        wb = c.wA[c.nw % 2]
        c.nw += 1
        load_w_slab(c, wb, w_dram, r0, nk, c0 + done, n)
        for tt in range(ntt):
            ps = c.next_ps()
            for k in range(nk):
                p.op("pe", lambda e, ps=ps, wb=wb, k=k, tt=tt, n=n: e.matmul(
                    ps[:, 0:n], act[:, k, tt * 128:(tt + 1) * 128], wb[:, k, 0:n],
                    start=(k == 0), stop=(k == nk - 1)),
                    reads=[wb, act], writes=[ps])
            evac(tt, done, n, ps)
        done += n


def out_proj(c, w_out, natt):
    p = c.p

    def evac(j, tc, ps):
        ts = slice(tc * TC, (tc + 1) * TC)
        p.op("dve", lambda e: e.tensor_tensor(c.xs[:, j, ts], ps[:, :], c.xs[:, j, ts], ALU.add),
             reads=[ps, (c.xs, (j, tc))], writes=[(c.xs, (j, tc))])
    lin_fm(c, c.hT, natt, TB // TC, w_out, 0, 0, D, evac)


def mlp(c, w_up, w_dn):
    p = c.p
    ntc = TB // TC
    for g in range(DFF // 512):
        wu = c.wA[c.nw % 2]
        wd = c.wB[c.nw % 2]
        c.nw += 1
        load_w_slab(c, wu, w_up, 0, 16, g * 512, 512)
        srcd = w_dn.t[g * 512:(g + 1) * 512, :].rearrange("(k p) n -> p k n", p=128)
        p.dma("pool", wd[:, :, :], srcd, reads=[w_dn], writes=[wd], chan=wd.name)
        for tc in range(ntc):
            ts = slice(tc * TC, (tc + 1) * TC)
            for hc in range(4):
                ps = c.next_ps()
                for k in range(16):
                    p.op("pe", lambda e, ps=ps, wu=wu, k=k, hc=hc, ts=ts: e.matmul(
                        ps[:, :], wu[:, k, hc * 128:(hc + 1) * 128], c.hT[:, k, ts],
                        start=(k == 0), stop=(k == 15)),
                        reads=[wu, c.hT], writes=[ps])
                sf = c.stgf[hc % 2]
                p.op("act", lambda e, ps=ps, sf=sf: e.activation(sf[:, :], ps[:, :], AF.Relu),
                     reads=[ps], writes=[sf])
                p.op("dve", lambda e, sf=sf, hc=hc, ts=ts: e.tensor_tensor(c.uT[:, hc, ts], sf[:, :], sf[:, :], ALU.mult),
                     reads=[sf], writes=[(c.uT, (hc, tc))])
            for o in range(16):
                ps = c.next_psY()
                for hc in range(4):
                    p.op("pe", lambda e, ps=ps, wd=wd, hc=hc, o=o, ts=ts: e.matmul(
                        ps[:, :], wd[:, hc, o * 128:(o + 1) * 128], c.uT[:, hc, ts],
                        start=(hc == 0), stop=(hc == 3)),
                        reads=[wd, (c.uT, (hc, tc))], writes=[ps])
                p.op("dve", lambda e, ps=ps, o=o, ts=ts: e.tensor_tensor(c.xs[:, o, ts], ps[:, :], c.xs[:, o, ts], ALU.add),
                     reads=[ps, (c.xs, (o, tc))], writes=[(c.xs, (o, tc))])


EVEN_SPEC = [("lat", "qlat", 512, 1.0), ("lat", "kvlat", 512, 1.0),
             ("rope", "kr2", 128, 1.0),
             ("fm", "bq", 1024, 128 ** -0.5), ("fm", "bk", 1024, 1.0), ("tm", "bv", 1024, 1.0)]
ODD_SPEC = [("fm", "dq", 1536, 128 ** -0.5), ("fm", "dk", 1536, 1.0), ("tm", "dv", 1536, 1.0),
            ("fm", "sq", 1024, 64 ** -0.5), ("fm", "sk2", 256, 1.0), ("tm", "sv", 128, 1.0)]
UQ_SPEC = [("fm", "qn", 1024, 192 ** -0.5), ("rope", "qr", 512, 192 ** -0.5)]
UKV_SPEC = [("fm", "kn", 1024, 1.0), ("tm", "mv", 1024, 1.0)]


class Split:
    def __init__(self, kind, bufs, chunk):
        self.kind = kind
        self.bufs = bufs
        self.chunk = chunk

    def loc(self, i0):
        return self.bufs[i0 // self.chunk], i0 % self.chunk


def spec_cols(spec):
    return sum(n * (2 if k == "rope" else 1) for (k, _, n, _) in spec)


def run_spec(c, spec, act, nk, w_dram, outs, tok0, lat=None, cs=None, col0=0):
    p = c.p
    ntc = TB // TC
    col = col0
    for (kind, name, ncols, scale) in spec:
        if kind == "fm":
            od = outs[name]

            def evac(j, tc, ps, od=od, scale=scale, name=name):
                st = c.next_stg()
                p.op("act", lambda e: e.activation(st[:, 0:TC], ps[:, :], AF.Copy, scale=scale),
                     reads=[ps], writes=[st])
                ob_, r_ = od.loc(j * 128)
                p.dma("sp", ob_.t[r_:r_ + 128, tok0 + tc * TC: tok0 + (tc + 1) * TC], st[:, 0:TC],
                      reads=[st], writes=[(ob_, (j, tok0, tc))], chan=st.name)
            lin_fm(c, act, nk, ntc, w_dram, 0, col, ncols, evac)
            col += ncols
        elif kind == "lat":
            lb = lat

            def evac(j, tc, ps, lb=lb):
                p.op("act", lambda e: e.activation(lb[:, j, tc * TC:(tc + 1) * TC], ps[:, :], AF.Copy),
                     reads=[ps], writes=[(lb, (j, tc))])
            lin_fm(c, act, nk, ntc, w_dram, 0, col, ncols, evac)
            col += ncols
        elif kind == "rope":
            od = outs[name]
            nch = ncols // 128
            ropeb = c.ropeb

            def evac(j, tc, ps, od=od, scale=scale, nch=nch):
                ts = slice(tc * TC, (tc + 1) * TC)
                if j % 2 == 0:
                    p.op("dve", lambda e: e.tensor_tensor(ropeb[:, 0, ts], ps[:, :], cs[0][:, ts], ALU.mult),
                         reads=[ps, cs[0]], writes=[(ropeb, tc)])
                else:
                    jj = j // 2
                    sf = c.stgf[jj % 2]
                    p.op("dve", lambda e: e.tensor_tensor(sf[:, :], ps[:, :], cs[1][:, ts], ALU.mult),
                         reads=[ps, cs[1]], writes=[sf])
                    p.op("dve", lambda e: e.tensor_tensor(sf[:, :], sf[:, :], ropeb[:, 0, ts], ALU.add),
                         reads=[sf, (ropeb, tc)], writes=[sf])
                    st = c.next_stg()
                    p.op("act", lambda e: e.activation(st[:, 0:TC], sf[:, :], AF.Copy, scale=scale),
                         reads=[sf], writes=[st])
                    ob_, r_ = od.loc(jj * 128)
                    p.dma("sp", ob_.t[r_:r_ + 128, tok0 + tc * TC: tok0 + (tc + 1) * TC], st[:, 0:TC],
                          reads=[st], writes=[(ob_, (jj, tok0, tc))], chan=st.name)
            lin_fm(c, act, nk, ntc, w_dram, 0, col, 2 * ncols, evac)
            col += 2 * ncols
        elif kind == "tm":
            od = outs[name]

            def evac(tt, coff, n, ps, od=od):
                st = c.next_stg()
                p.op("act", lambda e: e.activation(st[:, 0:n], ps[:, 0:n], AF.Copy),
                     reads=[ps], writes=[st])
                ob_, c_ = od.loc(coff)
                p.dma("sp", ob_.t[tok0 + tt * 128: tok0 + (tt + 1) * 128, c_:c_ + n], st[:, 0:n],
                      reads=[st], writes=[(ob_, (tt, tok0, coff))], chan=st.name)
            lin_tm(c, act, nk, TB // 128, w_dram, 0, col, ncols, evac)
            col += ncols
    return col


EVEN_A1 = [("lat", "qlat", 512, 1.0)]
EVEN_A2 = [("lat", "kvlat", 512, 1.0)]
EVEN_A3 = [("rope", "kr2", 128, 1.0),
           ("fm", "bq", 1024, 128 ** -0.5), ("fm", "bk", 1024, 1.0), ("tm", "bv", 1024, 1.0)]
W_IN_EVEN_COLS = 512 + 512 + 256 + 3072
W_IN_ODD_COLS = 1536 * 3 + 1024 + 256 + 128
OUT_SHAPES_EVEN = {"qn": ("fm", 1024), "qr": ("fm", 512), "kn": ("fm", 1024), "kr2": ("fm", 128), "mv": ("tm", 1024),
                   "bq": ("fm", 1024), "bk": ("fm", 1024), "bv": ("tm", 1024)}
OUT_SHAPES_ODD = {"dq": ("fm", 1536), "dk": ("fm", 1536), "dv": ("tm", 1536),
                  "sq": ("fm", 1024), "sk2": ("fm", 256), "sv": ("tm", 128)}
TCORE = 2048


def emit_L(p, c, i, io, nblk=TCORE // TB, last=None):
    T = TCORE
    if last is None:
        last = (i == 4)
    xT = io["xT"]
    msel = io["msel"]
    if i > 0:
        even_prev = (i - 1) % 2 == 0
        natt = 16 if even_prev else 12
        atG = io["atG"]
        R = 1024 if even_prev else 768
        load_gain(c, io["g_mlp"], 0, 16)
    if not last:
        even = (i % 2 == 0)
        load_gain(c, io["g_att"], 16, 16)
        if even:
            load_gain(c, io["g_q"], 32, 4)
            load_gain(c, io["g_kv"], 36, 4)
    else:
        load_gain(c, io["g_fin"], 16, 16)
    outs = io.get("outs", {})
    ntc = TB // TC
    for blk in range(nblk):
        tok0 = blk * TB
        tsl = slice(tok0, tok0 + TB)
        for k4 in range(4):
            p.dma("sp", c.xs[:, 4 * k4:4 * k4 + 4, :], xT.t[k4 * 512:(k4 + 1) * 512, tsl].rearrange("(k p) t -> p k t", p=128),
                  reads=[xT], writes=[c.xs], chan="xs")
        if i > 0:
            def src_rows(k):
                if even_prev:
                    h = k % 8
                    base = 0 if k < 8 else 512
                    return (h // 4) * R + base + (h % 4) * 128
                if k < 4:
                    return (k // 2) * R + (k % 2) * 128
                kk = k - 4
                return (kk // 4) * R + 256 + (kk % 4) * 128
            for half in range(2):
                ks = list(range(half * (natt // 2), (half + 1) * (natt // 2)))
                for j, k in enumerate(ks):
                    rk, lr = divmod(src_rows(k), R)
                    ag = atG[lr // 256]
                    r0 = rk * 256 + lr % 256
                    p.dma("sp", c.hT[:, k, :], ag.t[r0:r0 + 128, tok0:tok0 + TB], reads=[ag], writes=[(c.hT, ("ld", k))], chan="hTld%d" % (k % 4))
                    p.dma("sp", c.candB[:, j, :], ag.t[r0:r0 + 128, 2048 + tok0:2048 + tok0 + TB], reads=[ag], writes=[(c.candB, j)], chan="cBld%d" % (j % 4))
                n = len(ks)
                p.op("dve", lambda e, n=n: e.tensor_scalar(c.candB[:, 0:n, :], c.candB[:, 0:n, :], msel[:, 1:2], None, ALU.mult),
                     reads=[c.candB, msel], writes=[c.candB])
                p.op("dve", lambda e, n=n, k0=ks[0]: e.scalar_tensor_tensor(c.hT[:, k0:k0 + n, :], c.hT[:, k0:k0 + n, :], msel[:, 0:1], c.candB[:, 0:n, :], ALU.mult, ALU.add),
                     reads=[c.hT, c.candB, msel], writes=[c.hT])
            out_proj(c, io["w_out"], natt)
            rmsnorm_fm(c, c.xs, 16, 0, c.hT, D, ntc)
            mlp(c, io["w_up"], io["w_dn"])
        if not last:
            rmsnorm_fm(c, c.xs, 16, 16, c.hT, D, ntc)
            w_in = io["w_in"]
            if even:
                p.dma("sp", c.cosT[:, :], io["cosd"].t[:, tsl], reads=[io["cosd"]], writes=[c.cosT], chan="cosT")
                p.dma("sp", c.sinT[:, :], io["sind"].t[:, tsl], reads=[io["sind"]], writes=[c.sinT], chan="sinT")
                cs = (c.cosT, c.sinT)
                col = run_spec(c, EVEN_A1, c.hT, 16, w_in, outs, tok0, lat=c.lat, col0=0)
                rmsnorm_fm(c, c.lat, 4, 32, c.latn, 512, ntc)
                run_spec(c, UQ_SPEC, c.latn, 4, io["w_uq"], outs, tok0, cs=cs)
                col = run_spec(c, EVEN_A2, c.hT, 16, w_in, outs, tok0, lat=c.lat, col0=col)
                rmsnorm_fm(c, c.lat, 4, 36, c.latn, 512, ntc)
                run_spec(c, UKV_SPEC, c.latn, 4, io["w_ukv"], outs, tok0)
                run_spec(c, EVEN_A3, c.hT, 16, w_in, outs, tok0, cs=cs, col0=col)
            else:
                run_spec(c, ODD_SPEC, c.hT, 16, w_in, outs, tok0)
            xo = io["xo"]
            for k4 in range(4):
                p.dma("sp", xo.t[k4 * 512:(k4 + 1) * 512, tsl].rearrange("(k p) t -> p k t", p=128), c.xs[:, 4 * k4:4 * k4 + 4, :],
                      reads=[c.xs], writes=[(xo, (blk, k4))], chan="xs_st")
        else:
            yo = io["yo"]
            for tc in range(ntc):
                ts = slice(tc * TC, (tc + 1) * TC)
                for k in range(16):
                    sq = c.sq[c.nsq % 2]
                    c.nsq += 1
                    p.op("act", lambda e, sq=sq, k=k, ts=ts: e.activation(sq[:, :], c.xs[:, k, ts], AF.Square),
                         reads=[c.xs], writes=[sq])
                    p.op("pe", lambda e, sq=sq, k=k: e.matmul(c.psN[:, :], c.ones[:, :], sq[:, :], start=(k == 0), stop=(k == 15)),
                         reads=[sq, c.ones], writes=[c.psN])
                p.op("dve", lambda e: e.tensor_scalar(c.rstd[:, :], c.psN[:, :], 1.0 / D, EPS, ALU.mult, ALU.add),
                     reads=[c.psN], writes=[c.rstd])
                p.op("act", lambda e: e.activation(c.rstd[:, :], c.rstd[:, :], AF.Sqrt),
                     reads=[c.rstd], writes=[c.rstd])
                p.op("dve", lambda e: e.reciprocal(c.rstd[:, :], c.rstd[:, :]),
                     reads=[c.rstd], writes=[c.rstd])
                for k in range(16):
                    sf = c.stgf[k % 2]
                    p.op("dve", lambda e, k=k, ts=ts, sf=sf: e.scalar_tensor_tensor(
                        sf[:, :], c.xs[:, k, ts], c.gains[:, 16 + k:17 + k], c.rstd[:, :], ALU.mult, ALU.mult),
                        reads=[c.xs, c.rstd, c.gains], writes=[sf])
                    p.dma("sp", yo.t[k * 128:(k + 1) * 128, tok0 + tc * TC:tok0 + (tc + 1) * TC], sf[:, :],
                          reads=[sf], writes=[(yo, (blk, k, tc))], chan=sf.name)


S = 4096
NEG = -30000.0


class ACtx:
    def __init__(self, p, ps, ones, msel):
        self.p = p
        sb = lambda n, sh, dt: p.sb("A", n, sh, dt, base=4096)
        self.sb = sb
        self.psS = ps[0:2]
        self.psO = ps[2:4]
        self.psL = ps[4:6]
        self.psG = ps[6]
        self.pb = [sb("pb%d" % i, [128, 512], BF16) for i in range(3)]
        self.ones = ones
        self.msel = msel
        self.rl = sb("rl", [128, 512], F32)
        self.ob = [sb("ob%d" % i, [128, 512], BF16) for i in range(2)]
        self.qs = [sb("qs%d" % i, [128, S], BF16) for i in range(2)]
        self.ks = [sb("ks%d" % i, [128, S], BF16) for i in range(2)]
        self.vs = [sb("vs%d" % i, [128, S // 128, 128], BF16) for i in range(2)]
        self.cand = sb("cand", [128, S], BF16)
        self.candv = self.cand.view("candv", self.cand.t[:, :].rearrange("p (t d) -> p t d", d=128))
        self.nS = 0
        self.nO = 0
        self.npb = 0
        self.nob = 0
        self.nld = 0

    def blend(self, dst_ap_fn, cand_ap_fn, dst, cand):
        p = self.p
        m = self.msel
        p.op("pool", lambda e: e.tensor_scalar(cand_ap_fn(), cand_ap_fn(), m[:, 1:2], None, ALU.mult), reads=[cand, m], writes=[cand])
        p.op("dve", lambda e: e.scalar_tensor_tensor(dst_ap_fn(), dst_ap_fn(), m[:, 0:1], cand_ap_fn(), ALU.mult, ALU.add),
             reads=[dst, cand, m], writes=[dst])

    def load_fm(self, dst, Gs, rows_per_rank, rowA, rowB, nrows=128):
        p = self.p
        rpr = min(512, rows_per_rank)
        for r in range(2):
            cs = slice(r * 2048, (r + 1) * 2048)
            G = Gs[rowA // rpr]
            ra = r * rpr + rowA % rpr
            p.dma("sp", dst[0:nrows, cs], G.t[ra:ra + nrows, :], reads=[G], writes=[(dst, ("ld", r))], chan="ldA%d" % r)
            if rowB is not None:
                G = Gs[rowB // rpr]
                rb = r * rpr + rowB % rpr
                p.dma("sp", self.cand[0:nrows, cs], G.t[rb:rb + nrows, :], reads=[G], writes=[(self.cand, ("ld", r))], chan="ldB%d" % r)
        if rowB is not None:
            self.blend(lambda: dst[0:nrows, :], lambda: self.cand[0:nrows, :], dst, self.cand)


def dense_causal_head(c, qs, ks, vs, out_dram, row0, rope=None, aux=None, nchunks=S // 512):
    p = c.p

    def chunk(ch):
        cs = slice(ch * 512, (ch + 1) * 512)
        nkt = 4 * ch + 4
        O = c.psO[c.nO % 2]
        L = c.psL[c.nO % 2]
        c.nO += 1

        def qk(kt):
            Sb = c.psS[c.nS % 2]
            c.nS += 1
            ksl = slice(kt * 128, (kt + 1) * 128)
            last = (rope is None and aux is None)
            p.op("pe", lambda e: e.matmul(Sb[:, :], ks[:, ksl], qs[:, cs], start=True, stop=last),
                 reads=[ks, qs], writes=[Sb])
            if rope is not None:
                qrs, krs, po = rope
                p.op("pe", lambda e: e.matmul(Sb[:, :], krs[po:po + 64, ksl], qrs[po:po + 64, cs], start=False, stop=(aux is None)),
                     reads=[krs, qrs], writes=[Sb])
            if aux is not None:
                QA, KA, nr = aux
                p.op("pe", lambda e: e.matmul(Sb[:, :], KA[0:nr, ksl], QA[0:nr, cs], start=False, stop=True),
                     reads=[KA, QA], writes=[Sb])
            pb = c.pb[c.npb % 3]
            c.npb += 1
            p.op("act", lambda e: e.activation(pb[:, :], Sb[:, :], AF.Exp), reads=[Sb], writes=[pb])
            if kt >= 4 * ch:
                j = kt - 4 * ch
                p.op("pool", lambda e: e.affine_select(pb[:, :], pb[:, :], [[1, 512]], ALU.is_ge, 0.0,
                                                       base=-128 * j, channel_multiplier=-1),
                     reads=[pb], writes=[pb])
            return pb

        def pv(kt, pb):
            p.op("pe", lambda e: e.matmul(O[:, :], vs[:, kt, :], pb[:, :], start=(kt == 0), stop=(kt == nkt - 1)),
                 reads=[vs, pb], writes=[O])
            p.op("pe", lambda e: e.matmul(L[:, :], c.ones[:, :], pb[:, :], start=(kt == 0), stop=(kt == nkt - 1)),
                 reads=[c.ones, pb], writes=[L])

        cur = qk(0)
        for kt in range(nkt):
            nxt = qk(kt + 1) if kt + 1 < nkt else None
            pv(kt, cur)
            cur = nxt
        ob = c.ob[c.nob % 2]
        c.nob += 1
        p.op("dve", lambda e: e.reciprocal(c.rl[:, :], L[:, :]), reads=[L], writes=[c.rl])
        p.op("dve", lambda e: e.tensor_tensor(ob[:, :], O[:, :], c.rl[:, :], ALU.mult), reads=[O, c.rl], writes=[ob])
        od_ = out_dram[row0 // 256]
        rr = row0 % 256
        p.dma("sp", od_.t[rr:rr + 128, cs], ob[:, :], reads=[ob], writes=[(od_, (row0, ch))], chan=ob.name)

    for ch in range(nchunks):
        chunk(ch)


def moba_gating(c, qs, ks, QA, g):
    p = c.p
    p.op("dve", lambda e: e.tensor_reduce(g.kmf[:, :], ks[:, :].rearrange("p (n j) -> p n j", j=256), AX.X, ALU.add),
         reads=[ks], writes=[g.kmf])
    p.op("act", lambda e: e.activation(g.kmf[:, :], g.kmf[:, :], AF.Copy, scale=1.0 / 256), reads=[g.kmf], writes=[g.kmf])
    p.op("dve", lambda e: e.tensor_copy(g.kmh[:, :], g.kmf[:, :]), reads=[g.kmf], writes=[g.kmh])
    p.op("dve", lambda e: e.tensor_tensor(g.kml[:, :], g.kmf[:, :], g.kmh[:, :], ALU.subtract), reads=[g.kmf, g.kmh], writes=[g.kml])
    p.op("dve", lambda e: e.memset(g.g16[:, :], -1.0e30), writes=[g.g16])
    def tile(qt):
        own = qt // 2
        qsl = slice(qt * 128, (qt + 1) * 128)
        if own > 0:
            p.op("pe", lambda e: e.matmul(c.psG[:, 0:16], qs[:, qsl], g.kmh[:, :], start=True, stop=False),
                 reads=[qs, g.kmh], writes=[c.psG])
            p.op("pe", lambda e: e.matmul(c.psG[:, 0:16], qs[:, qsl], g.kml[:, :], start=False, stop=True),
                 reads=[qs, g.kml], writes=[c.psG])
            p.op("dve", lambda e: e.tensor_copy(g.g16[:, 0:own], c.psG[:, 0:own]), reads=[c.psG], writes=[g.g16])
        p.op("dve", lambda e: e.max(g.m8[:, :], g.g16[:, :]), reads=[g.g16], writes=[g.m8])
        p.op("dve", lambda e: e.tensor_scalar(g.mb[:, :], g.g16[:, :], g.m8[:, 2:3], NEG, ALU.is_lt, ALU.mult),
             reads=[g.g16, g.m8], writes=[g.mb])
        p.op("dve", lambda e: e.memset(g.mb[:, own:own + 1], 0.0), reads=[g.mb], writes=[g.mb])
        p.op("pe", lambda e: e.transpose(c.psG[0:16, 128:256], g.mb[:, :], g.ident[:, :]),
             reads=[g.mb, g.ident], writes=[c.psG])
        p.op("act", lambda e: e.activation(QA[0:16, qsl], c.psG[0:16, 128:256], AF.Copy),
             reads=[c.psG], writes=[(QA, ("m", qt))])

    for qt in range(S // 128):
        tile(qt)


class GBufs:
    def __init__(self, c):
        sb = c.sb
        self.kmf = sb("kmf", [128, 16], F32)
        self.kmh = sb("kmh", [128, 16], BF16)
        self.kml = sb("kml", [128, 16], BF16)
        self.g16 = sb("g16", [128, 16], F32)
        self.m8 = sb("m8", [128, 8], F32)
        self.mb = sb("mb", [128, 16], F32)
        self.ident = sb("ident_sb", [128, 128], F32)


def emit_A_even(p, c, io, nh=4, nchunks=S // 512):
    if not hasattr(c, "even_bufs"):
        c.even_bufs = (GBufs(c), c.sb("qrs", [128, S], BF16), c.sb("krs", [128, S], BF16), c.sb("KAs", [36, S], BF16),
                       [c.sb("QA%d" % i, [36, S], BF16) for i in range(2)])
    g, qrs, krs, KA, QA = c.even_bufs
    atT = io["atT"]
    c.load_fm(krs, io["kr2"], 128, 0, None)
    p.dma("sp", KA[:, :], io["KA"][:, :], reads=[io["KA"]], writes=[KA])
    p.dma("sp", g.ident[:, :], io["ident"][:, :], reads=[io["ident"]], writes=[g.ident])
    nb = 0

    def load_v(v_, Gs, colA, colB):
        GA, ca = Gs[colA // 512], colA % 512
        GB, cb = Gs[colB // 512], colB % 512
        p.dma("sp", v_[:, :, :], GA.t[:, ca:ca + 128].rearrange("(t p) d -> p t d", p=128), reads=[GA], writes=[v_], chan="ldvA")
        p.dma("sp", c.candv[:, :, :], GB.t[:, cb:cb + 128].rearrange("(t p) d -> p t d", p=128), reads=[GB], writes=[c.candv], chan="ldvB")
        c.blend(lambda: v_[:, :, :], lambda: c.candv[:, :, :], v_, c.candv)

    for h in range(nh):
        q_, k_, v_ = c.qs[nb % 2], c.ks[nb % 2], c.vs[nb % 2]
        nb += 1
        c.load_fm(q_, io["qn"], 1024, h * 128, (nh + h) * 128)
        c.load_fm(k_, io["kn"], 1024, h * 128, (nh + h) * 128)
        load_v(v_, io["mv"], h * 128, (nh + h) * 128)
        if h % 2 == 0:
            c.load_fm(qrs, io["qr"], 512, (h // 2) * 128, (nh // 2 + h // 2) * 128)
        dense_causal_head(c, q_, k_, v_, atT, h * 128, rope=(qrs, krs, (h % 2) * 64), nchunks=nchunks)
    for h in range(nh):
        q_, k_, v_ = c.qs[nb % 2], c.ks[nb % 2], c.vs[nb % 2]
        QA_ = QA[nb % 2]
        nb += 1
        c.load_fm(q_, io["bq"], 1024, h * 128, (nh + h) * 128)
        c.load_fm(k_, io["bk"], 1024, h * 128, (nh + h) * 128)
        load_v(v_, io["bv"], h * 128, (nh + h) * 128)
        p.op("dve", lambda e, QA_=QA_: e.memset(QA_[:, :], 0.0), writes=[QA_])
        p.dma("sp", QA_[32:36, :], io["QAc"].t[h, :, :], reads=[io["QAc"]], writes=[(QA_, "al")])
        moba_gating(c, q_, k_, QA_, g)
        dense_causal_head(c, q_, k_, v_, atT, (nh + h) * 128, aux=(QA_, KA, 36), nchunks=nchunks)


DIL_D = (1, 4, 16)


def emit_A_odd(p, c, io, ngroups=3, nslots=2, nswa=8):
    dq, dk, dv, sq, sk2, sv = io["dq"], io["dk"], io["dv"], io["sq"], io["sk2"], io["sv"]
    atT = io["atT"]
    qs, ks, vs = c.qs, c.ks, c.vs
    if not hasattr(c, "odd_bufs"):
        c.odd_bufs = (c.sb("Oacc", [128, S], F32), c.sb("Lacc", [128, S], F32), c.sb("KAws", [4, 14 * 128], BF16),
                      c.sb("QAws", [4, 14 * 2 * 128], BF16), c.sb("maskss", [128, 3 * 128], BF16),
                      c.sb("identbs", [128, 128], BF16), c.sb("es", [128, 8], F32))
    Oacc, Lacc, KAw, QAw, masks, identb, es = c.odd_bufs
    for (sb_, dd) in ((KAw, io["KAw"]), (QAw, io["QAw"]), (masks, io["masks"]), (identb, io["identb"]), (es, io["sinks"])):
        p.dma("sp", sb_[:, :], dd[:, :], reads=[dd], writes=[sb_])
    p.op("act", lambda e: e.activation(es[:, :], es[:, :], AF.Exp), reads=[es], writes=[es])

    def win_tile(q_ap, k_aps, v_aps, hw, prev_mask, O_ap, L_ap, O, L):
        items = [(w, k_aps[w], v_aps[w]) for w in (0, 1) if k_aps[w] is not None]
        for n, (w, k_ap, v_ap) in enumerate(items):
            Sb = c.psS[c.nS % 2]
            c.nS += 1
            mi = 0 if w == 1 else prev_mask
            p.op("pe", lambda e, Sb=Sb, k_ap=k_ap: e.matmul(Sb[:, 0:128], k_ap, q_ap, start=True, stop=False),
                 reads=[c.kq_bufs[0], c.kq_bufs[1]], writes=[Sb])
            p.op("pe", lambda e, Sb=Sb, w=w: e.matmul(Sb[:, 0:128], KAw[0:4, hw * 128:(hw + 1) * 128],
                                                     QAw[0:4, (hw * 2 + w) * 128:(hw * 2 + w + 1) * 128], start=False, stop=False),
                 reads=[KAw, QAw], writes=[Sb])
            p.op("pe", lambda e, Sb=Sb, mi=mi: e.matmul(Sb[:, 0:128], identb[:, :], masks[:, mi * 128:(mi + 1) * 128], start=False, stop=True),
                 reads=[identb, masks], writes=[Sb])
            pb = c.pb[c.npb % 3]
            c.npb += 1
            p.op("act", lambda e, Sb=Sb, pb=pb: e.activation(pb[:, 0:128], Sb[:, 0:128], AF.Exp), reads=[Sb], writes=[pb])
            first, last = (n == 0), (n == len(items) - 1)
            p.op("pe", lambda e, pb=pb, v_ap=v_ap, first=first, last=last: e.matmul(O_ap, v_ap, pb[:, 0:128], start=first, stop=last),
                 reads=[c.kq_bufs[2], pb], writes=[O])
            p.op("pe", lambda e, pb=pb, first=first, last=last: e.matmul(L_ap, c.ones[:, :], pb[:, 0:128], start=first, stop=last),
                 reads=[c.ones, pb], writes=[L])

    nb = 0
    for jl in range(nslots):
        for g in range(ngroups):
            d = DIL_D[g]
            hw = jl * 3 + g
            q_, k_, v_ = qs[nb % 2], ks[nb % 2], vs[nb % 2]
            nb += 1
            c.kq_bufs = (k_, q_, v_)
            hdA = g * 4 + jl
            hdB = g * 4 + 2 + jl
            c.load_fm(q_, dq, 1536, hdA * 128, hdB * 128)
            c.load_fm(k_, dk, 1536, hdA * 128, hdB * 128)
            nbk = S // (128 * d)
            for r in range(d):
                for (dst_, hd_, nm) in ((v_, hdA, "A"), (c.candv, hdB, "B")):
                    dvb = dv[(hd_ * 128) // 512]
                    cc_ = (hd_ * 128) % 512
                    src = dvb.t[:, cc_:cc_ + 128].rearrange("(b p r) e -> r p b e", p=128, r=d)[r]
                    p.dma("sp", dst_[:, r * nbk:(r + 1) * nbk, :], src, reads=[dvb], writes=[(dst_, r)], chan="ldv%s_%d" % (nm, r % 4))
            c.blend(lambda v_=v_: v_[:, :, :], lambda: c.candv[:, :, :], v_, c.candv)
            bat = min(4, nbk)
            for r in range(d):
                for b0 in range(0, nbk, bat):
                    O = c.psO[c.nO % 2]
                    L = c.psL[c.nO % 2]
                    c.nO += 1
                    for jb in range(bat):
                        b = b0 + jb
                        col = lambda bb: slice(r + d * 128 * bb, r + d * 128 * bb + d * 127 + 1, d)
                        q_ap = q_[:, col(b)]
                        k_aps = [k_[:, col(b - 1)] if b > 0 else None, k_[:, col(b)]]
                        v_aps = [v_[:, r * nbk + b - 1, :] if b > 0 else None, v_[:, r * nbk + b, :]]
                        win_tile(q_ap, k_aps, v_aps, hw, 1, O[:, jb * 128:(jb + 1) * 128], L[:, jb * 128:(jb + 1) * 128], O, L)
                    n = bat * 128
                    tsl = slice(r + d * 128 * b0, r + d * 128 * b0 + d * (bat * 128 - 1) + 1, d)
                    if g == 0:
                        p.op("act", lambda e, O=O, tsl=tsl, n=n: e.activation(Oacc[:, tsl], O[:, 0:n], AF.Copy), reads=[O], writes=[Oacc])
                        p.op("act", lambda e, L=L, tsl=tsl, n=n: e.activation(Lacc[:, tsl], L[:, 0:n], AF.Copy), reads=[L], writes=[Lacc])
                    else:
                        p.op("dve", lambda e, O=O, tsl=tsl, n=n: e.tensor_tensor(Oacc[:, tsl], O[:, 0:n], Oacc[:, tsl], ALU.add), reads=[O, Oacc], writes=[Oacc])
                        p.op("dve", lambda e, L=L, tsl=tsl, n=n: e.tensor_tensor(Lacc[:, tsl], L[:, 0:n], Lacc[:, tsl], ALU.add), reads=[L, Lacc], writes=[Lacc])
        for ch in range(S // 512):
            cs = slice(ch * 512, (ch + 1) * 512)
            ob = c.ob[c.nob % 2]
            c.nob += 1
            p.op("dve", lambda e, cs=cs: e.reciprocal(c.rl[:, :], Lacc[:, cs]), reads=[Lacc], writes=[c.rl])
            p.op("dve", lambda e, cs=cs, ob=ob: e.tensor_tensor(ob[:, :], Oacc[:, cs], c.rl[:, :], ALU.mult), reads=[Oacc, c.rl], writes=[ob])
            p.dma("sp", atT[0].t[jl * 128:(jl + 1) * 128, cs], ob[:, :], reads=[ob], writes=[(atT[0], (jl, ch))], chan=ob.name)
    if nswa:
        k_ = ks[nb % 2]
        v_ = vs[nb % 2]
        c.load_fm(k_, sk2, 256, 0, 128)
        for jj in range(2):
            p.dma("sp", v_[:, :, jj * 64:(jj + 1) * 64], sv[0].t[:, 0:64].rearrange("(t p) e -> p t e", p=128), reads=[sv[0]], writes=[(v_, jj)], chan="ldvA_%d" % jj)
            p.dma("sp", c.candv[:, :, jj * 64:(jj + 1) * 64], sv[0].t[:, 64:128].rearrange("(t p) e -> p t e", p=128), reads=[sv[0]], writes=[(c.candv, jj)], chan="ldvB_%d" % jj)
        c.blend(lambda: v_[:, :, :], lambda: c.candv[:, :, :], v_, c.candv)
        for hl in range(nswa):
            if hl % 2 == 0:
                q_ = qs[(hl // 2) % 2]
                c.load_fm(q_, sq, 1024, (hl // 2) * 128, 512 + (hl // 2) * 128)
            c.kq_bufs = (k_, q_, v_)
            po = (hl % 2) * 64
            hw = 6 + hl
            for b0 in range(0, S // 128, 4):
                O = c.psO[c.nO % 2]
                L = c.psL[c.nO % 2]
                c.nO += 1
                for jb in range(4):
                    b = b0 + jb
                    col = lambda bb: slice(128 * bb, 128 * (bb + 1))
                    q_ap = q_[po:po + 64, col(b)]
                    k_aps = [k_[po:po + 64, col(b - 1)] if b > 0 else None, k_[po:po + 64, col(b)]]
                    v_aps = [v_[:, b - 1, :] if b > 0 else None, v_[:, b, :]]
                    win_tile(q_ap, k_aps, v_aps, hw, 2, O[:, jb * 128:(jb + 1) * 128], L[:, jb * 128:(jb + 1) * 128], O, L)
                cs = slice(b0 * 128, b0 * 128 + 512)
                ob = c.ob[c.nob % 2]
                c.nob += 1
                p.op("dve", lambda e, L=L, hl=hl, po=po: e.tensor_scalar(c.rl[po:po + 64, :], L[po:po + 64, :], es[po:po + 64, hl:hl + 1], None, ALU.add),
                     reads=[L, es], writes=[c.rl])
                p.op("dve", lambda e, po=po: e.reciprocal(c.rl[po:po + 64, :], c.rl[po:po + 64, :]), reads=[c.rl], writes=[c.rl])
                p.op("dve", lambda e, O=O, ob=ob, po=po: e.tensor_tensor(ob[po:po + 64, :], O[po:po + 64, :], c.rl[po:po + 64, :], ALU.mult),
                     reads=[O, c.rl], writes=[ob])
                ro_ = 256 + hl * 64
                p.dma("sp", atT[ro_ // 256].t[ro_ % 256:ro_ % 256 + 64, cs], ob[po:po + 64, :], reads=[ob], writes=[(atT[ro_ // 256], ("s", hl, b0))], chan=ob.name)

import numpy as np
import ml_dtypes

def gain_layout(g):
    return np.ascontiguousarray(np.asarray(g, np.float32).reshape(-1, 128).T)

def prep_w_in_odd(w):
    cd = 1536
    dq, dk, dv = w[:, :cd], w[:, cd:2 * cd], w[:, 2 * cd:3 * cd]
    o = 3 * cd
    sq = w[:, o:o + 1024]
    sk = w[:, o + 1024:o + 1152]
    sv = w[:, o + 1152:o + 1280]
    sk2 = np.concatenate([sk[:, :64], sk[:, :64], sk[:, 64:], sk[:, 64:]], axis=1)
    return np.ascontiguousarray(np.concatenate([dq, dk, dv, sq, sk2, sv], axis=1))

def _swap(a):
    n = a.shape[1] // 64
    b = a.reshape(a.shape[0], n, 2, 32)[:, :, ::-1, :]
    return b.reshape(a.shape[0], n * 64)

def prep_w_in_even(w):
    qlat, kvlat, kpe = w[:, :512], w[:, 512:1024], w[:, 1024:1088]
    rest = w[:, 1088:]
    krx = np.concatenate([kpe, kpe], axis=1)
    krs = _swap(krx)
    return np.ascontiguousarray(np.concatenate([qlat, kvlat, krx, krs, rest], axis=1))

def prep_w_uq(w):
    w3 = w.reshape(512, 8, 192)
    nope = w3[:, :, :128].reshape(512, 1024)
    rope = w3[:, :, 128:].reshape(512, 512)
    rsw = _swap(rope)
    parts = [nope]
    for j in range(4):
        parts += [rope[:, j * 128:(j + 1) * 128], rsw[:, j * 128:(j + 1) * 128]]
    return np.ascontiguousarray(np.concatenate(parts, axis=1))

def prep_w_ukv(w):
    w3 = w.reshape(512, 8, 256)
    return np.ascontiguousarray(np.concatenate([w3[:, :, :128].reshape(512, 1024), w3[:, :, 128:].reshape(512, 1024)], axis=1))

def rope_tables(S=4096):
    half = 32
    inv = (10000.0 ** (-np.arange(half, dtype=np.float32) / half)).astype(np.float32)
    ang = np.arange(S, dtype=np.float32)[None, :] * inv[:, None]
    cos, sin = np.cos(ang).astype(np.float32), np.sin(ang).astype(np.float32)
    C = np.concatenate([cos, cos, cos, cos], axis=0)
    Sg = np.concatenate([-sin, sin, -sin, sin], axis=0)
    return np.ascontiguousarray(C), np.ascontiguousarray(Sg)

def make_KA(S=4096):
    KA = np.zeros((36, S), np.float32)
    s = np.arange(S)
    KA[s // 256, s] = 1.0
    KA[32] = 1.0
    KA[33] = 1.0
    KA[34] = 128.0 * (s // 128)
    KA[35] = s % 128
    return KA.astype(ml_dtypes.bfloat16)

def make_QAc(heads, nheads_total=8, S=4096):
    t = np.arange(S)
    out = np.zeros((len(heads), 4, S), np.float32)
    for i, h in enumerate(heads):
        slope = 2.0 ** (-8.0 * (h + 1) / nheads_total)
        out[i, 0] = -slope * 128.0 * (t // 128)
        out[i, 1] = -slope * (t % 128)
        out[i, 2] = slope
        out[i, 3] = slope
    return out.astype(ml_dtypes.bfloat16)

def _hilo(x):
    hi = np.float32(x).astype(ml_dtypes.bfloat16)
    lo = (np.float32(x) - hi.astype(np.float32)).astype(ml_dtypes.bfloat16)
    return hi, lo

def make_win_consts(hh):
    cs = []
    for jl in range(2):
        for g in range(3):
            hd = g * 4 + (2 * hh + jl)
            slope = 2.0 ** (-8.0 * (hd + 1) / 12)
            cs.append(slope * (1, 4, 16)[g])
    for hl in range(8):
        hq = 8 * hh + hl
        cs.append(2.0 ** (-8.0 * (hq + 1) / 16))
    KAw = np.zeros((4, 14, 128), np.float32)
    QAw = np.zeros((4, 14, 2, 128), np.float32)
    sl = np.arange(128, dtype=np.float32)
    for i, cval in enumerate(cs):
        hi, lo = _hilo(cval)
        hi, lo = np.float32(hi), np.float32(lo)
        KAw[0, i] = hi; KAw[1, i] = lo; KAw[2, i] = sl; KAw[3, i] = sl
        for w, off in ((0, 128.0), (1, 0.0)):
            QAw[0, i, w] = -(sl + off); QAw[1, i, w] = -(sl + off); QAw[2, i, w] = hi; QAw[3, i, w] = lo
    s_ = np.arange(128)[:, None]; t_ = np.arange(128)[None, :]
    NEGV = -30000.0
    m_own = np.where(t_ >= s_, 0.0, NEGV)
    m_p128 = np.where(t_ <= s_, 0.0, NEGV)
    m_p127 = np.where(t_ < s_, 0.0, NEGV)
    masks = np.concatenate([m_own, m_p128, m_p127], axis=1)
    bf = ml_dtypes.bfloat16
    return (KAw.reshape(4, -1).astype(bf), QAw.reshape(4, -1).astype(bf), masks.astype(bf), np.eye(128, dtype=np.float32).astype(bf))


PAIRS = [[0, 1], [2, 3], [4, 5], [6, 7]]
NLAYER = 4


def build_fused(NLAYER=NLAYER, with_attn=True, ncoll=99):
    nc = bass.Bass("TRN2", target_bir_lowering=False)
    p = Prog(nc)
    p.arena_init(208896)
    ps = [p.psum("ps%d" % i, [128, 512], F32) for i in range(8)]
    ones = p.sb("C", "ones", [128, 128], BF16)
    gains = p.sb("C", "gains", [128, 64], F32)
    msel = p.sb("C", "msel", [128, 2], F32)
    p.op("dve", lambda e: e.memset(ones[:, :], 1.0), writes=[ones])
    di = lambda n, sh, dt=F32: p.dram(n, sh, dt, kind="ExternalInput")
    T = TCORE
    mseld = di("msel", [128, 2])
    p.dma("sp", msel[:, :], mseld[:, :], reads=[mseld], writes=[msel])
    lc = LCtx(p, ps, ones, gains)
    ac = ACtx(p, ps, ones, msel)
    xT = di("xT", [D, T])
    cosd = di("cosd", [128, T]); sind = di("sind", [128, T])
    KAd = di("KA", [36, S], BF16); QAcd = di("QAc", [4, 4, S], BF16); identd = di("ident", [128, 128])
    KAwd = di("KAw", [4, 14 * 128], BF16); QAwd = di("QAw", [4, 14 * 2 * 128], BF16)
    maskd = di("masks", [128, 3 * 128], BF16); identbd = di("identb", [128, 128], BF16)
    yo = p.dram("yo", [D, T], F32, kind="ExternalOutput")
    atG = None
    x_cur = xT
    for i in range(NLAYER + 1):
        io = {"xT": x_cur, "msel": msel}
        if i > 0:
            l = i - 1
            natt = 16 if l % 2 == 0 else 12
            io.update(atG=atG, w_out=di("w_out%d" % l, [natt * 128, D]), g_mlp=di("g_mlp%d" % l, [128, 16]),
                      w_up=di("w_up%d" % l, [D, DFF]), w_dn=di("w_dn%d" % l, [DFF, D]))
        if i < NLAYER:
            even = i % 2 == 0
            io.update(g_att=di("g_att%d" % i, [128, 16]), w_in=di("w_in%d" % i, [D, W_IN_EVEN_COLS if even else W_IN_ODD_COLS]))
            shapes = OUT_SHAPES_EVEN if even else OUT_SHAPES_ODD
            outs = {}
            for nm, (kind, n) in shapes.items():
                ch = min(512, n)
                bufs = [p.dram("o%d_%s_%d" % (i, nm, j), [ch, T] if kind == "fm" else [T, ch], BF16) for j in range(n // ch)]
                outs[nm] = Split(kind, bufs, ch)
            io["outs"] = outs
            if even:
                io.update(g_q=di("g_q%d" % i, [128, 4]), g_kv=di("g_kv%d" % i, [128, 4]), w_uq=di("w_uq%d" % i, [512, 2048]),
                          w_ukv=di("w_ukv%d" % i, [512, 2048]), cosd=cosd, sind=sind)
            io["xo"] = p.dram("x%d" % (i + 1), [D, T], F32)
        else:
            io.update(g_fin=di("g_fin", [128, 16]), yo=yo)
        emit_L(p, lc, i, io, last=(i == NLAYER))
        if i == NLAYER:
            break
        x_cur = io["xo"]
        gath = {}
        for nm, (kind, n) in shapes.items():
            gl = []
            for j, ob_ in enumerate(outs[nm].bufs):
                ch = outs[nm].chunk
                gb = p.dram("g%d_%s_%d" % (i, nm, j), [2 * ch, T] if kind == "fm" else [2 * T, ch], BF16)
                p.coll("AllGather", PAIRS, ob_, gb)
                gl.append(gb)
            gath[nm] = gl
        p.barrier()
        nat = 4 if even else 3
        at = [p.dram("at%d_%d" % (i, j), [256, S], BF16) for j in range(nat)]
        aio = dict(gath)
        aio["atT"] = at
        if even:
            aio.update(KA=KAd, QAc=QAcd, ident=identd)
            emit_A_even(p, ac, aio)
        else:
            aio.update(KAw=KAwd, QAw=QAwd, masks=maskd, identb=identbd, sinks=di("sinks%d" % i, [128, 8]))
            emit_A_odd(p, ac, aio)
        atG = []
        for j in range(nat):
            gb = p.dram("atG%d_%d" % (i, j), [512, S], BF16)
            p.coll("AllGather", PAIRS, at[j], gb)
            atG.append(gb)
        p.barrier()
    st = p.emit()
    st["arena"] = dict(p.arena_off)
    p.close()
    return nc, st


_FUSED = {}


def kernel(x, attn_norm, mlp_norm, w_up, w_down, ev_w_in, ev_q_norm, ev_w_uq, ev_kv_norm,
           ev_w_ukv, ev_w_out, od_w_in, od_sinks, od_w_out, final_norm):
    from concourse.bass_utils import run_bass_kernel_spmd
    A = lambda a: np.ascontiguousarray(np.asarray(a))
    x = A(x)
    B, SS, DD = x.shape
    cores = list(range(8))
    if "nc" not in _FUSED:
        _FUSED["nc"] = build_fused()[0]
    nc = _FUSED["nc"]
    C_, S_ = rope_tables()
    common = dict(KA=make_KA(), ident=np.eye(128, dtype=np.float32), g_fin=gain_layout(final_norm))
    for l in range(NLAYER):
        common["g_mlp%d" % l] = gain_layout(mlp_norm[l])
        common["w_up%d" % l] = A(w_up[l])
        common["w_dn%d" % l] = A(w_down[l])
        common["g_att%d" % l] = gain_layout(attn_norm[l])
        if l % 2 == 0:
            j = l // 2
            common["w_out%d" % l] = A(ev_w_out[j])
            common["w_in%d" % l] = prep_w_in_even(A(ev_w_in[j]))
            common["g_q%d" % l] = gain_layout(ev_q_norm[j])
            common["g_kv%d" % l] = gain_layout(ev_kv_norm[j])
            common["w_uq%d" % l] = prep_w_uq(A(ev_w_uq[j]))
            common["w_ukv%d" % l] = prep_w_ukv(A(ev_w_ukv[j]))
        else:
            j = l // 2
            common["w_out%d" % l] = A(od_w_out[j])
            common["w_in%d" % l] = prep_w_in_odd(A(od_w_in[j]))
    in_maps = []
    for c in cores:
        b, r = c // 2, c % 2
        m = dict(common)
        m["xT"] = A(x[b, r * 2048:(r + 1) * 2048, :].T)
        m["cosd"] = A(C_[:, r * 2048:(r + 1) * 2048])
        m["sind"] = A(S_[:, r * 2048:(r + 1) * 2048])
        m["msel"] = A(np.tile(np.array([[1.0 - r, float(r)]], np.float32), (128, 1)))
        m["QAc"] = make_QAc(list(range(4 * r, 4 * r + 4)))
        KAw, QAw, masks, identb = make_win_consts(r)
        m.update(KAw=KAw, QAw=QAw, masks=masks, identb=identb)
        for l in (1, 3):
            sk = np.asarray(od_sinks[l // 2], np.float32)
            m["sinks%d" % l] = A(np.tile(sk[None, 8 * r:8 * r + 8], (128, 1)))
        in_maps.append(m)
    res = run_bass_kernel_spmd(nc, in_maps, core_ids=cores).results
    out = np.empty((B, SS, DD), np.float32)
    for c in cores:
        out[c // 2, (c % 2) * 2048:(c % 2 + 1) * 2048, :] = res[c]["yo"].T
    return out
```

```python
import numpy as np
import concourse.bass as bass
import concourse.mybir as mybir
from contextlib import ExitStack

F32 = mybir.dt.float32
BF16 = mybir.dt.bfloat16
ALU = mybir.AluOpType
AF = mybir.ActivationFunctionType
AX = mybir.AxisListType

ENGS = ("pe", "act", "dve", "pool", "sp")


class Buf:
    def __init__(self, name, t):
        self.name = name
        self.t = t
        self.st = {}

    def __getitem__(self, idx):
        return self.t[idx]

    def view(self, name, ap):
        v = Buf(name, ap)
        v.st = self.st
        return v


class Op:
    __slots__ = ("eng", "fn", "reads", "writes", "dma", "chan", "deps", "sig", "ordv", "inc")

    def __init__(self, eng, fn, reads, writes, dma=False, chan=None):
        self.eng = eng
        self.fn = fn
        self.reads = reads
        self.writes = writes
        self.dma = dma
        self.chan = chan
        self.deps = ()
        self.sig = False
        self.ordv = 0
        self.inc = 16


def _norm(lst):
    out = []
    for x in lst:
        if isinstance(x, tuple):
            out.append(x)
        else:
            out.append((x, None))
    return out


class Prog:
    def __init__(self, nc, same_engine_raw=True):
        self.nc = nc
        self.ops = []
        self.es = ExitStack()
        self.same_engine_raw = same_engine_raw
        self.engobj = {"pe": nc.tensor, "act": nc.scalar, "dve": nc.vector,
                       "pool": nc.gpsimd, "sp": nc.sync}
        self.nbuf = 0

    def sbuf(self, name, shape, dt):
        t = self.es.enter_context(self.nc.sbuf_tensor(name, list(shape), dt))
        return Buf(name, t)

    def psum(self, name, shape, dt):
        t = self.es.enter_context(self.nc.psum_tensor(name, list(shape), dt))
        return Buf(name, t)

    def dram(self, name, shape, dt, kind="Internal"):
        t = self.nc.dram_tensor(name, list(shape), dt, kind=kind)
        return Buf(name, t.ap())

    def op(self, eng, fn, reads=(), writes=()):
        self.ops.append(Op(eng, fn, _norm(reads), _norm(writes)))

    def barrier(self):
        self.ops.append(Op("bar", None, [], []))

    def coll(self, kind, groups, in_buf, out_buf, chan="cc"):
        def fn(e, in_buf=in_buf, out_buf=out_buf):
            return e.collective_compute(kind, ALU.bypass, replica_groups=groups,
                                        ins=[in_buf.t[:, :].opt()], outs=[out_buf.t[:, :].opt()])
        o = Op("pool", fn, [(in_buf, None)], [(out_buf, None)], dma=True, chan=chan)
        o.inc = 1
        self.ops.append(o)

    def arena_init(self, nbytes):
        self.arena = self.es.enter_context(self.nc.sbuf_tensor("arena", [128, nbytes], mybir.dt.uint8))
        self.arena_off = {}

    def sb(self, group, name, shape, dt, base=0):
        esz = 4 if dt == F32 or dt == mybir.dt.int32 else 2
        n = 1
        for d in shape[1:]:
            n *= d
        nb = (n * esz + 31) // 32 * 32
        off = self.arena_off.get(group, base)
        self.arena_off[group] = off + nb
        assert off + nb <= self.arena.shape[1], (group, name, off + nb)
        ap = self.arena[0:shape[0], off:off + n * esz].bitcast(dt)
        if len(shape) == 3:
            ap = ap.rearrange("p (a b) -> p a b", b=shape[2])
        return Buf(name, ap)

    def dma(self, q, out_ap, in_ap, reads=(), writes=(), chan=None, **kw):
        def fn(e, out_ap=out_ap, in_ap=in_ap, kw=kw):
            return e.dma_start(out=out_ap, in_=in_ap, **kw)
        writes = _norm(writes)
        if chan is None:
            chan = "%s:%s" % (writes[0][0].name, writes[0][1])
        self.ops.append(Op(q, fn, _norm(reads), writes, dma=True, chan=chan))

    @staticmethod
    def _states(buf, key):
        st = buf.st
        if key is None:
            if None not in st:
                st[None] = [None, {}]
            return list(st.values())
        res = []
        if None in st:
            res.append(st[None])
        if key not in st:
            st[key] = [None, {}]
        res.append(st[key])
        return res

    def _analyze(self):
        last = {}
        bar = set()
        for i, o in enumerate(self.ops):
            if o.eng == "bar":
                bar = set(last.values())
                o.deps = ()
                continue
            deps = set(bar)
            ek = ("dma", o.chan) if o.dma else o.eng
            last[ek] = i
            for (b, k) in o.reads:
                for s in self._states(b, k):
                    if s[0] is not None:
                        deps.add(s[0])
            for (b, k) in o.writes:
                for s in self._states(b, k):
                    if s[0] is not None:
                        deps.add(s[0])
                    deps.update(s[1].values())
            deps.discard(i)
            for (b, k) in o.reads:
                if k is None:
                    for s in self._states(b, None):
                        s[1][ek] = i
                else:
                    self._states(b, k)[-1][1][ek] = i
            for (b, k) in o.writes:
                if k is None:
                    b.st.clear()
                    b.st[None] = [i, {}]
                else:
                    s = self._states(b, k)[-1]
                    s[0] = i
                    s[1] = {}
            o.deps = deps

    def emit(self):
        nc = self.nc
        self._analyze()
        ops = self.ops
        for i, o in enumerate(ops):
            if o.eng == "bar":
                continue
            per = {}
            for d in o.deps:
                od = ops[d]
                if od.dma:
                    key = ("dma", od.chan)
                else:
                    if od.eng == o.eng and not o.dma:
                        if od.eng == "pe" or not self.same_engine_raw:
                            continue
                        raw = any((b.st is rb.st and (k is None or rk is None or k == rk))
                                  for (b, k) in od.writes for (rb, rk) in o.reads)
                        if not raw:
                            continue
                    key = ("eng", od.eng)
                if key not in per or per[key] < d:
                    per[key] = d
            o.deps = sorted(per.values())
            for d in o.deps:
                ops[d].sig = True
        cnt = {e: 0 for e in ENGS}
        for o in ops:
            if o.dma or o.eng == "bar":
                continue
            if o.sig:
                cnt[o.eng] += 1
                o.ordv = cnt[o.eng]
        sems = {e: self.es.enter_context(nc.semaphore("s_" + e)) for e in ENGS}
        chans = {}
        chan_total = {}
        known = {e: {} for e in ENGS}
        nwait = 0
        for i, o in enumerate(ops):
            if o.eng == "bar":
                continue
            e = self.engobj[o.eng]
            kn = known[o.eng]
            for d in o.deps:
                od = ops[d]
                if od.dma:
                    key = ("dma", od.chan)
                    val = chan_total[od.chan]
                    sem = chans[od.chan]
                else:
                    key = ("eng", od.eng)
                    val = od.ordv
                    sem = sems[od.eng]
                if kn.get(key, 0) >= val:
                    continue
                e.wait_ge(sem, val)
                nwait += 1
                kn[key] = val
            ins = o.fn(e)
            if o.dma:
                ch = o.chan
                if ch not in chans:
                    chans[ch] = self.es.enter_context(nc.semaphore("d_" + str(ch)))
                    chan_total[ch] = 0
                chan_total[ch] += o.inc
                ins.then_inc(chans[ch], o.inc)
            elif o.sig:
                ins.then_inc(sems[o.eng], 1)
        for ch, tot in chan_total.items():
            self.engobj["sp"].wait_ge(chans[ch], tot)
        self.stats = dict(n_ops=len(ops), n_wait=nwait, n_chan=len(chans),
                          sig={e: cnt[e] for e in ENGS})
        return self.stats

    def close(self):
        self.es.close()


D = 2048
DFF = 8192
TB = 1024
TC = 512
EPS = 1e-6
NEG = -30000.0


class LCtx:
    def __init__(self, p, ps, ones, gains):
        self.p = p
        sb = lambda n, sh, dt: p.sb("L", n, sh, dt, base=4096)
        self.xs = sb("xs", [128, 16, TB], F32)
        self.hT = sb("hT", [128, 16, TB], BF16)
        self.wA = [sb("wA%d" % i, [128, 16, 512], BF16) for i in range(2)]
        self.wB = [sb("wB%d" % i, [128, 4, 2048], BF16) for i in range(2)]
        self.uT = sb("uT", [128, 4, TB], BF16)
        self.sq = [sb("sq%d" % i, [128, 512], BF16) for i in range(2)]
        self.rstd = sb("rstd", [128, 512], F32)
        self.ones = ones
        self.stg = [sb("stg%d" % i, [128, 1024], BF16) for i in range(2)]
        self.stgf = [sb("stgf%d" % i, [128, 512], F32) for i in range(2)]
        self.gains = gains
        self.ps = ps[0:4]
        self.psN = ps[4]
        self.psY = ps[5:7]
        self.nps = 0
        self.nY = 0
        self.nw = 0
        self.nsq = 0
        self.nstg = 0
        self.nalt = 0
        self.lat = self.wB[0].view("lat", self.wB[0].t[:, :, :].bitcast(F32))
        self.candB = self.wB[1].view("candB", self.wB[1].t[:, :, :].rearrange("p a (b c) -> p (a b) c", c=TB))
        self.latn = sb("latn", [128, 4, TB], BF16)
        self.ropeb = sb("ropeb", [128, 1, TB], F32)
        self.cosT = sb("cosT", [128, TB], F32)
        self.sinT = sb("sinT", [128, TB], F32)

    def next_ps(self):
        b = self.ps[self.nps % 4]
        self.nps += 1
        return b

    def next_psY(self):
        b = self.psY[self.nY % 2]
        self.nY += 1
        return b

    def next_stg(self):
        b = self.stg[self.nstg % 2]
        self.nstg += 1
        return b


def load_gain(c, g_dram, col0, nk):
    p = c.p
    p.dma("sp", c.gains[:, col0:col0 + nk], g_dram[:, :], reads=[g_dram], writes=[(c.gains, col0)],
          chan="gains%d" % col0)


def rmsnorm_fm(c, src, nk, gcol, dst, dfeat, ntc, src_key=None):
    p = c.p
    for tc in range(ntc):
        ts = slice(tc * TC, (tc + 1) * TC)
        for k in range(nk):
            sq = c.sq[c.nsq % 2]
            c.nsq += 1
            p.op("act", lambda e, sq=sq, k=k, ts=ts: e.activation(sq[:, :], src[:, k, ts], AF.Square),
                 reads=[src], writes=[sq])
            p.op("pe", lambda e, sq=sq, k=k: e.matmul(c.psN[:, :], c.ones[:, :], sq[:, :], start=(k == 0), stop=(k == nk - 1)),
                 reads=[sq, c.ones], writes=[c.psN])
        p.op("dve", lambda e: e.tensor_scalar(c.rstd[:, :], c.psN[:, :], 1.0 / dfeat, EPS, ALU.mult, ALU.add),
             reads=[c.psN], writes=[c.rstd])
        p.op("act", lambda e: e.activation(c.rstd[:, :], c.rstd[:, :], AF.Sqrt),
             reads=[c.rstd], writes=[c.rstd])
        p.op("dve", lambda e: e.reciprocal(c.rstd[:, :], c.rstd[:, :]),
             reads=[c.rstd], writes=[c.rstd])
        for k in range(nk):
            p.op("dve", lambda e, k=k, ts=ts: e.scalar_tensor_tensor(
                dst[:, k, ts], src[:, k, ts], c.gains[:, gcol + k:gcol + k + 1], c.rstd[:, :], ALU.mult, ALU.mult),
                reads=[src, c.rstd, c.gains], writes=[(dst, ("n", k, tc))])


def load_w_slab(c, wbuf, w_dram, r0, nk, c0, ncols):
    p = c.p
    src = w_dram.t[r0:r0 + nk * 128, c0:c0 + ncols].rearrange("(k p) n -> p k n", p=128)
    p.dma("pool", wbuf[:, 0:nk, 0:ncols], src, reads=[w_dram], writes=[wbuf], chan=wbuf.name)


def lin_fm(c, act, nk, ntc, w_dram, r0, c0, ncols, evac, act_keys=None):
    p = c.p
    done = 0
    while done < ncols:
        n = min(512, ncols - done)
        wb = c.wA[c.nw % 2]
        c.nw += 1
        load_w_slab(c, wb, w_dram, r0, nk, c0 + done, n)
        for jj in range(n // 128):
            j = (done // 128) + jj
            for tc in range(ntc):
                ps = c.next_ps()
                for k in range(nk):
                    p.op("pe", lambda e, ps=ps, wb=wb, k=k, jj=jj, tc=tc: e.matmul(
                        ps[:, :], wb[:, k, jj * 128:(jj + 1) * 128], act[:, k, tc * TC:(tc + 1) * TC],
                        start=(k == 0), stop=(k == nk - 1)),
                        reads=[wb, act], writes=[ps])
                evac(j, tc, ps)
        done += n


def lin_tm(c, act, nk, ntt, w_dram, r0, c0, ncols, evac):
    p = c.p
    done = 0
    while done < ncols:
        n = min(512, ncols - done)
        wb = c.wA[c.nw % 2]
        c.nw += 1
        load_w_slab(c, wb, w_dram, r0, nk, c0 + done, n)
        for tt in range(ntt):
            ps = c.next_ps()
            for k in range(nk):
                p.op("pe", lambda e, ps=ps, wb=wb, k=k, tt=tt, n=n: e.matmul(
                    ps[:, 0:n], act[:, k, tt * 128:(tt + 1) * 128], wb[:, k, 0:n],
                    start=(k == 0), stop=(k == nk - 1)),
                    reads=[wb, act], writes=[ps])
            evac(tt, done, n, ps)
        done += n


def out_proj(c, w_out, natt):
    p = c.p

    def evac(j, tc, ps):
        ts = slice(tc * TC, (tc + 1) * TC)
        p.op("dve", lambda e: e.tensor_tensor(c.xs[:, j, ts], ps[:, :], c.xs[:, j, ts], ALU.add),
             reads=[ps, (c.xs, (j, tc))], writes=[(c.xs, (j, tc))])
    lin_fm(c, c.hT, natt, TB // TC, w_out, 0, 0, D, evac)


def mlp(c, w_up, w_dn):
    p = c.p
    ntc = TB // TC
    for g in range(DFF // 512):
        wu = c.wA[c.nw % 2]
        wd = c.wB[c.nw % 2]
        c.nw += 1
        load_w_slab(c, wu, w_up, 0, 16, g * 512, 512)
        srcd = w_dn.t[g * 512:(g + 1) * 512, :].rearrange("(k p) n -> p k n", p=128)
        p.dma("pool", wd[:, :, :], srcd, reads=[w_dn], writes=[wd], chan=wd.name)
        for tc in range(ntc):
            ts = slice(tc * TC, (tc + 1) * TC)
            for hc in range(4):
                ps = c.next_ps()
                for k in range(16):
                    p.op("pe", lambda e, ps=ps, wu=wu, k=k, hc=hc, ts=ts: e.matmul(
                        ps[:, :], wu[:, k, hc * 128:(hc + 1) * 128], c.hT[:, k, ts],
                        start=(k == 0), stop=(k == 15)),
                        reads=[wu, c.hT], writes=[ps])
                sf = c.stgf[hc % 2]
                p.op("act", lambda e, ps=ps, sf=sf: e.activation(sf[:, :], ps[:, :], AF.Relu),
                     reads=[ps], writes=[sf])
                p.op("dve", lambda e, sf=sf, hc=hc, ts=ts: e.tensor_tensor(c.uT[:, hc, ts], sf[:, :], sf[:, :], ALU.mult),
                     reads=[sf], writes=[(c.uT, (hc, tc))])
            for o in range(16):
                ps = c.next_psY()
                for hc in range(4):
                    p.op("pe", lambda e, ps=ps, wd=wd, hc=hc, o=o, ts=ts: e.matmul(
                        ps[:, :], wd[:, hc, o * 128:(o + 1) * 128], c.uT[:, hc, ts],
                        start=(hc == 0), stop=(hc == 3)),
                        reads=[wd, (c.uT, (hc, tc))], writes=[ps])
                p.op("dve", lambda e, ps=ps, o=o, ts=ts: e.tensor_tensor(c.xs[:, o, ts], ps[:, :], c.xs[:, o, ts], ALU.add),
                     reads=[ps, (c.xs, (o, tc))], writes=[(c.xs, (o, tc))])


EVEN_SPEC = [("lat", "qlat", 512, 1.0), ("lat", "kvlat", 512, 1.0),
             ("rope", "kr2", 128, 1.0),
             ("fm", "bq", 1024, 128 ** -0.5), ("fm", "bk", 1024, 1.0), ("tm", "bv", 1024, 1.0)]
ODD_SPEC = [("fm", "dq", 1536, 128 ** -0.5), ("fm", "dk", 1536, 1.0), ("tm", "dv", 1536, 1.0),
            ("fm", "sq", 1024, 64 ** -0.5), ("fm", "sk2", 256, 1.0), ("tm", "sv", 128, 1.0)]
UQ_SPEC = [("fm", "qn", 1024, 192 ** -0.5), ("rope", "qr", 512, 192 ** -0.5)]
UKV_SPEC = [("fm", "kn", 1024, 1.0), ("tm", "mv", 1024, 1.0)]


class Split:
    def __init__(self, kind, bufs, chunk):
        self.kind = kind
        self.bufs = bufs
        self.chunk = chunk

    def loc(self, i0):
        return self.bufs[i0 // self.chunk], i0 % self.chunk


def spec_cols(spec):
    return sum(n * (2 if k == "rope" else 1) for (k, _, n, _) in spec)


def run_spec(c, spec, act, nk, w_dram, outs, tok0, lat=None, cs=None, col0=0):
    p = c.p
    ntc = TB // TC
    col = col0
    for (kind, name, ncols, scale) in spec:
        if kind == "fm":
            od = outs[name]

            def evac(j, tc, ps, od=od, scale=scale, name=name):
                st = c.next_stg()
                p.op("act", lambda e: e.activation(st[:, 0:TC], ps[:, :], AF.Copy, scale=scale),
                     reads=[ps], writes=[st])
                ob_, r_ = od.loc(j * 128)
                p.dma("sp", ob_.t[r_:r_ + 128, tok0 + tc * TC: tok0 + (tc + 1) * TC], st[:, 0:TC],
                      reads=[st], writes=[(ob_, (j, tok0, tc))], chan=st.name)
            lin_fm(c, act, nk, ntc, w_dram, 0, col, ncols, evac)
            col += ncols
        elif kind == "lat":
            lb = lat

            def evac(j, tc, ps, lb=lb):
                p.op("act", lambda e: e.activation(lb[:, j, tc * TC:(tc + 1) * TC], ps[:, :], AF.Copy),
                     reads=[ps], writes=[(lb, (j, tc))])
            lin_fm(c, act, nk, ntc, w_dram, 0, col, ncols, evac)
            col += ncols
        elif kind == "rope":
            od = outs[name]
            nch = ncols // 128
            ropeb = c.ropeb

            def evac(j, tc, ps, od=od, scale=scale, nch=nch):
                ts = slice(tc * TC, (tc + 1) * TC)
                if j % 2 == 0:
                    p.op("dve", lambda e: e.tensor_tensor(ropeb[:, 0, ts], ps[:, :], cs[0][:, ts], ALU.mult),
                         reads=[ps, cs[0]], writes=[(ropeb, tc)])
                else:
                    jj = j // 2
                    sf = c.stgf[jj % 2]
                    p.op("dve", lambda e: e.tensor_tensor(sf[:, :], ps[:, :], cs[1][:, ts], ALU.mult),
                         reads=[ps, cs[1]], writes=[sf])
                    p.op("dve", lambda e: e.tensor_tensor(sf[:, :], sf[:, :], ropeb[:, 0, ts], ALU.add),
                         reads=[sf, (ropeb, tc)], writes=[sf])
                    st = c.next_stg()
                    p.op("act", lambda e: e.activation(st[:, 0:TC], sf[:, :], AF.Copy, scale=scale),
                         reads=[sf], writes=[st])
                    ob_, r_ = od.loc(jj * 128)
                    p.dma("sp", ob_.t[r_:r_ + 128, tok0 + tc * TC: tok0 + (tc + 1) * TC], st[:, 0:TC],
                          reads=[st], writes=[(ob_, (jj, tok0, tc))], chan=st.name)
            lin_fm(c, act, nk, ntc, w_dram, 0, col, 2 * ncols, evac)
            col += 2 * ncols
        elif kind == "tm":
            od = outs[name]

            def evac(tt, coff, n, ps, od=od):
                st = c.next_stg()
                p.op("act", lambda e: e.activation(st[:, 0:n], ps[:, 0:n], AF.Copy),
                     reads=[ps], writes=[st])
                ob_, c_ = od.loc(coff)
                p.dma("sp", ob_.t[tok0 + tt * 128: tok0 + (tt + 1) * 128, c_:c_ + n], st[:, 0:n],
                      reads=[st], writes=[(ob_, (tt, tok0, coff))], chan=st.name)
            lin_tm(c, act, nk, TB // 128, w_dram, 0, col, ncols, evac)
            col += ncols
    return col


EVEN_A1 = [("lat", "qlat", 512, 1.0)]
EVEN_A2 = [("lat", "kvlat", 512, 1.0)]
EVEN_A3 = [("rope", "kr2", 128, 1.0),
           ("fm", "bq", 1024, 128 ** -0.5), ("fm", "bk", 1024, 1.0), ("tm", "bv", 1024, 1.0)]
W_IN_EVEN_COLS = 512 + 512 + 256 + 3072
W_IN_ODD_COLS = 1536 * 3 + 1024 + 256 + 128
OUT_SHAPES_EVEN = {"qn": ("fm", 1024), "qr": ("fm", 512), "kn": ("fm", 1024), "kr2": ("fm", 128), "mv": ("tm", 1024),
                   "bq": ("fm", 1024), "bk": ("fm", 1024), "bv": ("tm", 1024)}
OUT_SHAPES_ODD = {"dq": ("fm", 1536), "dk": ("fm", 1536), "dv": ("tm", 1536),
                  "sq": ("fm", 1024), "sk2": ("fm", 256), "sv": ("tm", 128)}
TCORE = 2048


def emit_L(p, c, i, io, nblk=TCORE // TB, last=None):
    T = TCORE
    if last is None:
        last = (i == 4)
    xT = io["xT"]
    msel = io["msel"]
    if i > 0:
        even_prev = (i - 1) % 2 == 0
        natt = 16 if even_prev else 12
        atG = io["atG"]
        R = 1024 if even_prev else 768
        load_gain(c, io["g_mlp"], 0, 16)
    if not last:
        even = (i % 2 == 0)
        load_gain(c, io["g_att"], 16, 16)
        if even:
            load_gain(c, io["g_q"], 32, 4)
            load_gain(c, io["g_kv"], 36, 4)
    else:
        load_gain(c, io["g_fin"], 16, 16)
    outs = io.get("outs", {})
    ntc = TB // TC
    for blk in range(nblk):
        tok0 = blk * TB
        tsl = slice(tok0, tok0 + TB)
        for k4 in range(4):
            p.dma("sp", c.xs[:, 4 * k4:4 * k4 + 4, :], xT.t[k4 * 512:(k4 + 1) * 512, tsl].rearrange("(k p) t -> p k t", p=128),
                  reads=[xT], writes=[c.xs], chan="xs")
        if i > 0:
            def src_rows(k):
                if even_prev:
                    h = k % 8
                    base = 0 if k < 8 else 512
                    return (h // 4) * R + base + (h % 4) * 128
                if k < 4:
                    return (k // 2) * R + (k % 2) * 128
                kk = k - 4
                return (kk // 4) * R + 256 + (kk % 4) * 128
            for half in range(2):
                ks = list(range(half * (natt // 2), (half + 1) * (natt // 2)))
                for j, k in enumerate(ks):
                    rk, lr = divmod(src_rows(k), R)
                    ag = atG[lr // 256]
                    r0 = rk * 256 + lr % 256
                    p.dma("sp", c.hT[:, k, :], ag.t[r0:r0 + 128, tok0:tok0 + TB], reads=[ag], writes=[(c.hT, ("ld", k))], chan="hTld%d" % (k % 4))
                    p.dma("sp", c.candB[:, j, :], ag.t[r0:r0 + 128, 2048 + tok0:2048 + tok0 + TB], reads=[ag], writes=[(c.candB, j)], chan="cBld%d" % (j % 4))
                n = len(ks)
                p.op("dve", lambda e, n=n: e.tensor_scalar(c.candB[:, 0:n, :], c.candB[:, 0:n, :], msel[:, 1:2], None, ALU.mult),
                     reads=[c.candB, msel], writes=[c.candB])
                p.op("dve", lambda e, n=n, k0=ks[0]: e.scalar_tensor_tensor(c.hT[:, k0:k0 + n, :], c.hT[:, k0:k0 + n, :], msel[:, 0:1], c.candB[:, 0:n, :], ALU.mult, ALU.add),
                     reads=[c.hT, c.candB, msel], writes=[c.hT])
            out_proj(c, io["w_out"], natt)
            rmsnorm_fm(c, c.xs, 16, 0, c.hT, D, ntc)
            mlp(c, io["w_up"], io["w_dn"])
        if not last:
            rmsnorm_fm(c, c.xs, 16, 16, c.hT, D, ntc)
            w_in = io["w_in"]
            if even:
                p.dma("sp", c.cosT[:, :], io["cosd"].t[:, tsl], reads=[io["cosd"]], writes=[c.cosT], chan="cosT")
                p.dma("sp", c.sinT[:, :], io["sind"].t[:, tsl], reads=[io["sind"]], writes=[c.sinT], chan="sinT")
                cs = (c.cosT, c.sinT)
                col = run_spec(c, EVEN_A1, c.hT, 16, w_in, outs, tok0, lat=c.lat, col0=0)
                rmsnorm_fm(c, c.lat, 4, 32, c.latn, 512, ntc)
                run_spec(c, UQ_SPEC, c.latn, 4, io["w_uq"], outs, tok0, cs=cs)
                col = run_spec(c, EVEN_A2, c.hT, 16, w_in, outs, tok0, lat=c.lat, col0=col)
                rmsnorm_fm(c, c.lat, 4, 36, c.latn, 512, ntc)
                run_spec(c, UKV_SPEC, c.latn, 4, io["w_ukv"], outs, tok0)
                run_spec(c, EVEN_A3, c.hT, 16, w_in, outs, tok0, cs=cs, col0=col)
            else:
                run_spec(c, ODD_SPEC, c.hT, 16, w_in, outs, tok0)
            xo = io["xo"]
            for k4 in range(4):
                p.dma("sp", xo.t[k4 * 512:(k4 + 1) * 512, tsl].rearrange("(k p) t -> p k t", p=128), c.xs[:, 4 * k4:4 * k4 + 4, :],
                      reads=[c.xs], writes=[(xo, (blk, k4))], chan="xs_st")
        else:
            yo = io["yo"]
            for tc in range(ntc):
                ts = slice(tc * TC, (tc + 1) * TC)
                for k in range(16):
                    sq = c.sq[c.nsq % 2]
                    c.nsq += 1
                    p.op("act", lambda e, sq=sq, k=k, ts=ts: e.activation(sq[:, :], c.xs[:, k, ts], AF.Square),
                         reads=[c.xs], writes=[sq])
                    p.op("pe", lambda e, sq=sq, k=k: e.matmul(c.psN[:, :], c.ones[:, :], sq[:, :], start=(k == 0), stop=(k == 15)),
                         reads=[sq, c.ones], writes=[c.psN])
                p.op("dve", lambda e: e.tensor_scalar(c.rstd[:, :], c.psN[:, :], 1.0 / D, EPS, ALU.mult, ALU.add),
                     reads=[c.psN], writes=[c.rstd])
                p.op("act", lambda e: e.activation(c.rstd[:, :], c.rstd[:, :], AF.Sqrt),
                     reads=[c.rstd], writes=[c.rstd])
                p.op("dve", lambda e: e.reciprocal(c.rstd[:, :], c.rstd[:, :]),
                     reads=[c.rstd], writes=[c.rstd])
                for k in range(16):
                    sf = c.stgf[k % 2]
                    p.op("dve", lambda e, k=k, ts=ts, sf=sf: e.scalar_tensor_tensor(
                        sf[:, :], c.xs[:, k, ts], c.gains[:, 16 + k:17 + k], c.rstd[:, :], ALU.mult, ALU.mult),
                        reads=[c.xs, c.rstd, c.gains], writes=[sf])
                    p.dma("sp", yo.t[k * 128:(k + 1) * 128, tok0 + tc * TC:tok0 + (tc + 1) * TC], sf[:, :],
                          reads=[sf], writes=[(yo, (blk, k, tc))], chan=sf.name)


S = 4096
NEG = -30000.0


class ACtx:
    def __init__(self, p, ps, ones, msel):
        self.p = p
        sb = lambda n, sh, dt: p.sb("A", n, sh, dt, base=4096)
        self.sb = sb
        self.psS = ps[0:2]
        self.psO = ps[2:4]
        self.psL = ps[4:6]
        self.psG = ps[6]
        self.psS4 = [ps[0], ps[1], ps[6], ps[7]]
        self.pb = [sb("pb%d" % i, [128, 512], BF16) for i in range(3)]
        self.ones = ones
        self.msel = msel
        self.rl = sb("rl", [128, 512], F32)
        self.ob = [sb("ob%d" % i, [128, 512], BF16) for i in range(2)]
        self.qs = [sb("qs%d" % i, [128, S], BF16) for i in range(2)]
        self.ks = [sb("ks%d" % i, [128, S], BF16) for i in range(2)]
        self.vs = [sb("vs%d" % i, [128, S // 128, 128], BF16) for i in range(2)]
        self.cmask = sb("cmask", [128, 4 * 512], BF16)
        self.identb = sb("identb_c", [128, 128], BF16)
        self.cand = sb("cand", [128, S], BF16)
        self.candv = self.cand.view("candv", self.cand.t[:, :].rearrange("p (t d) -> p t d", d=128))
        self.nS = 0
        self.nO = 0
        self.npb = 0
        self.nob = 0
        self.nld = 0

    def blend(self, dst_ap_fn, cand_ap_fn, dst, cand):
        p = self.p
        m = self.msel
        p.op("dve", lambda e: e.tensor_scalar(cand_ap_fn(), cand_ap_fn(), m[:, 1:2], None, ALU.mult), reads=[cand, m], writes=[cand])
        p.op("dve", lambda e: e.scalar_tensor_tensor(dst_ap_fn(), dst_ap_fn(), m[:, 0:1], cand_ap_fn(), ALU.mult, ALU.add),
             reads=[dst, cand, m], writes=[dst])

    def load_fm(self, dst, Gs, rows_per_rank, rowA, rowB, nrows=128):
        p = self.p
        rpr = min(512, rows_per_rank)
        for r in range(2):
            cs = slice(r * 2048, (r + 1) * 2048)
            G = Gs[rowA // rpr]
            ra = r * rpr + rowA % rpr
            p.dma("sp", dst[0:nrows, cs], G.t[ra:ra + nrows, :], reads=[G], writes=[(dst, ("ld", r))], chan="ldA%d" % r)
            if rowB is not None:
                G = Gs[rowB // rpr]
                rb = r * rpr + rowB % rpr
                p.dma("sp", self.cand[0:nrows, cs], G.t[rb:rb + nrows, :], reads=[G], writes=[(self.cand, ("ld", r))], chan="ldB%d" % r)
        if rowB is not None:
            self.blend(lambda: dst[0:nrows, :], lambda: self.cand[0:nrows, :], dst, self.cand)


def dense_causal_head(c, qs, ks, vs, out_dram, row0, rope=None, aux=None, nchunks=S // 512):
    p = c.p

    def chunk(ch):
        cs = slice(ch * 512, (ch + 1) * 512)
        nkt = 4 * ch + 4
        O = c.psO[c.nO % 2]
        L = c.psL[c.nO % 2]
        c.nO += 1

        def qk(kt):
            Sb = c.psS[c.nS % 2]
            c.nS += 1
            ksl = slice(kt * 128, (kt + 1) * 128)
            diag = kt >= 4 * ch
            mm = [(ks, qs, lambda e: (ks[:, ksl], qs[:, cs]))]
            if rope is not None:
                qrs, krs, po = rope
                mm.append((krs, qrs, lambda e: (krs[po:po + 64, ksl], qrs[po:po + 64, cs])))
            if aux is not None:
                QA, KA, nr = aux
                mm.append((KA, QA, lambda e: (KA[0:nr, ksl], QA[0:nr, cs])))
            if diag:
                j = kt - 4 * ch
                mm.append((c.identb, c.cmask, lambda e: (c.identb[:, :], c.cmask[:, j * 512:(j + 1) * 512])))
            for n, (b0, b1, f) in enumerate(mm):
                p.op("pe", lambda e, f=f, n=n: e.matmul(Sb[:, :], *f(e), start=(n == 0), stop=(n == len(mm) - 1)),
                     reads=[b0, b1], writes=[Sb])
            pb = c.pb[c.npb % 3]
            c.npb += 1
            p.op("act", lambda e: e.activation(pb[:, :], Sb[:, :], AF.Exp), reads=[Sb], writes=[pb])
            return pb

        def pv(kt, pb):
            p.op("pe", lambda e: e.matmul(O[:, :], vs[:, kt, :], pb[:, :], start=(kt == 0), stop=(kt == nkt - 1)),
                 reads=[vs, pb], writes=[O])
            p.op("pe", lambda e: e.matmul(L[:, :], c.ones[:, :], pb[:, :], start=(kt == 0), stop=(kt == nkt - 1)),
                 reads=[c.ones, pb], writes=[L])

        cur = qk(0)
        for kt in range(nkt):
            nxt = qk(kt + 1) if kt + 1 < nkt else None
            pv(kt, cur)
            cur = nxt
        ob = c.ob[c.nob % 2]
        c.nob += 1
        p.op("dve", lambda e: e.reciprocal(c.rl[:, :], L[:, :]), reads=[L], writes=[c.rl])
        p.op("dve", lambda e: e.tensor_tensor(ob[:, :], O[:, :], c.rl[:, :], ALU.mult), reads=[O, c.rl], writes=[ob])
        od_ = out_dram[row0 // 256]
        rr = row0 % 256
        p.dma("sp", od_.t[rr:rr + 128, cs], ob[:, :], reads=[ob], writes=[(od_, (row0, ch))], chan=ob.name)

    for ch in range(nchunks):
        chunk(ch)


def moba_gating(c, qs, ks, QA, g):
    p = c.p
    p.op("dve", lambda e: e.tensor_reduce(g.kmf[:, :], ks[:, :].rearrange("p (n j) -> p n j", j=256), AX.X, ALU.add),
         reads=[ks], writes=[g.kmf])
    p.op("act", lambda e: e.activation(g.kmf[:, :], g.kmf[:, :], AF.Copy, scale=1.0 / 256), reads=[g.kmf], writes=[g.kmf])
    p.op("dve", lambda e: e.tensor_copy(g.kmh[:, :], g.kmf[:, :]), reads=[g.kmf], writes=[g.kmh])
    p.op("dve", lambda e: e.tensor_tensor(g.kml[:, :], g.kmf[:, :], g.kmh[:, :], ALU.subtract), reads=[g.kmf, g.kmh], writes=[g.kml])
    p.op("dve", lambda e: e.memset(g.g16[:, :], -1.0e30), writes=[g.g16])
    def tile(qt):
        own = qt // 2
        qsl = slice(qt * 128, (qt + 1) * 128)
        if own > 0:
            p.op("pe", lambda e: e.matmul(c.psG[:, 0:16], qs[:, qsl], g.kmh[:, :], start=True, stop=False),
                 reads=[qs, g.kmh], writes=[c.psG])
            p.op("pe", lambda e: e.matmul(c.psG[:, 0:16], qs[:, qsl], g.kml[:, :], start=False, stop=True),
                 reads=[qs, g.kml], writes=[c.psG])
            p.op("dve", lambda e: e.tensor_copy(g.g16[:, 0:own], c.psG[:, 0:own]), reads=[c.psG], writes=[g.g16])
        p.op("dve", lambda e: e.max(g.m8[:, :], g.g16[:, :]), reads=[g.g16], writes=[g.m8])
        p.op("dve", lambda e: e.tensor_scalar(g.mb[:, :], g.g16[:, :], g.m8[:, 2:3], NEG, ALU.is_lt, ALU.mult),
             reads=[g.g16, g.m8], writes=[g.mb])
        p.op("dve", lambda e: e.memset(g.mb[:, own:own + 1], 0.0), reads=[g.mb], writes=[g.mb])
        p.op("pe", lambda e: e.transpose(c.psG[0:16, 128:256], g.mb[:, :], g.ident[:, :]),
             reads=[g.mb, g.ident], writes=[c.psG])
        p.op("act", lambda e: e.activation(QA[0:16, qsl], c.psG[0:16, 128:256], AF.Copy),
             reads=[c.psG], writes=[(QA, ("m", qt))])

    for qt in range(S // 128):
        tile(qt)


class GBufs:
    def __init__(self, c):
        sb = c.sb
        self.kmf = sb("kmf", [128, 16], F32)
        self.kmh = sb("kmh", [128, 16], BF16)
        self.kml = sb("kml", [128, 16], BF16)
        self.g16 = sb("g16", [128, 16], F32)
        self.m8 = sb("m8", [128, 8], F32)
        self.mb = sb("mb", [128, 16], F32)
        self.ident = sb("ident_sb", [128, 128], F32)


def emit_A_even(p, c, io, nh=4, nchunks=S // 512, piece_done=lambda j: None):
    if not hasattr(c, "even_bufs"):
        c.even_bufs = (GBufs(c), c.sb("qrs", [128, S], BF16), c.sb("krs", [128, S], BF16), c.sb("KAs", [36, S], BF16),
                       [c.sb("QA%d" % i, [36, S], BF16) for i in range(2)])
    g, qrs, krs, KA, QA = c.even_bufs
    atT = io["atT"]
    c.load_fm(krs, io["kr2"], 128, 0, None)
    p.dma("sp", KA[:, :], io["KA"][:, :], reads=[io["KA"]], writes=[KA])
    p.dma("sp", g.ident[:, :], io["ident"][:, :], reads=[io["ident"]], writes=[g.ident])
    nb = 0

    def load_v(v_, Gs, colA, colB):
        GA, ca = Gs[colA // 512], colA % 512
        GB, cb = Gs[colB // 512], colB % 512
        p.dma("sp", v_[:, :, :], GA.t[:, ca:ca + 128].rearrange("(t p) d -> p t d", p=128), reads=[GA], writes=[v_], chan="ldvA")
        p.dma("sp", c.candv[:, :, :], GB.t[:, cb:cb + 128].rearrange("(t p) d -> p t d", p=128), reads=[GB], writes=[c.candv], chan="ldvB")
        c.blend(lambda: v_[:, :, :], lambda: c.candv[:, :, :], v_, c.candv)

    for h in range(nh):
        q_, k_, v_ = c.qs[nb % 2], c.ks[nb % 2], c.vs[nb % 2]
        nb += 1
        c.load_fm(q_, io["qn"], 1024, h * 128, (nh + h) * 128)
        c.load_fm(k_, io["kn"], 1024, h * 128, (nh + h) * 128)
        load_v(v_, io["mv"], h * 128, (nh + h) * 128)
        if h % 2 == 0:
            c.load_fm(qrs, io["qr"], 512, (h // 2) * 128, (nh // 2 + h // 2) * 128)
        dense_causal_head(c, q_, k_, v_, atT, h * 128, rope=(qrs, krs, (h % 2) * 64), nchunks=nchunks)
        if h % 2 == 1:
            piece_done(h // 2)
    for h in range(nh):
        q_, k_, v_ = c.qs[nb % 2], c.ks[nb % 2], c.vs[nb % 2]
        QA_ = QA[nb % 2]
        nb += 1
        c.load_fm(q_, io["bq"], 1024, h * 128, (nh + h) * 128)
        c.load_fm(k_, io["bk"], 1024, h * 128, (nh + h) * 128)
        load_v(v_, io["bv"], h * 128, (nh + h) * 128)
        p.op("dve", lambda e, QA_=QA_: e.memset(QA_[:, :], 0.0), writes=[QA_])
        p.dma("sp", QA_[32:36, :], io["QAc"].t[h, :, :], reads=[io["QAc"]], writes=[(QA_, "al")])
        moba_gating(c, q_, k_, QA_, g)
        dense_causal_head(c, q_, k_, v_, atT, (nh + h) * 128, aux=(QA_, KA, 36), nchunks=nchunks)
        if h % 2 == 1:
            piece_done(nh // 2 + h // 2)


DIL_D = (1, 4, 16)


def emit_A_odd(p, c, io, ngroups=3, nslots=2, nswa=8, piece_done=lambda j: None):
    dq, dk, dv, sq, sk2, sv = io["dq"], io["dk"], io["dv"], io["sq"], io["sk2"], io["sv"]
    atT = io["atT"]
    qs, ks, vs = c.qs, c.ks, c.vs
    if not hasattr(c, "odd_bufs"):
        c.odd_bufs = (c.sb("Oacc", [128, S], F32), c.sb("Lacc", [128, S], F32), c.sb("KAws", [4, 14 * 128], BF16),
                      c.sb("QAws", [4, 14 * 2 * 128], BF16), c.sb("maskss", [128, 3 * 128], BF16),
                      c.sb("identbs", [128, 128], BF16), c.sb("es", [128, 8], F32))
    Oacc, Lacc, KAw, QAw, masks, identb, es = c.odd_bufs
    for (sb_, dd) in ((KAw, io["KAw"]), (QAw, io["QAw"]), (masks, io["masks"]), (identb, io["identb"]), (es, io["sinks"])):
        p.dma("sp", sb_[:, :], dd[:, :], reads=[dd], writes=[sb_])
    p.op("act", lambda e: e.activation(es[:, :], es[:, :], AF.Exp), reads=[es], writes=[es])

    pend = []

    def flush(depth):
        while len(pend) > depth:
            pend.pop(0)()

    def win_tile(q_ap, k_aps, v_aps, hw, prev_mask, O_ap, L_ap, O, L):
        items = [(w, k_aps[w], v_aps[w]) for w in (0, 1) if k_aps[w] is not None]
        kb, qb, vb = c.kq_bufs
        for n, (w, k_ap, v_ap) in enumerate(items):
            Sb = c.psS4[c.nS % 4]
            sq_ = 0
            c.nS += 1
            ssl = slice(0, 128)
            mi = 0 if w == 1 else prev_mask
            p.op("pe", lambda e, Sb=Sb, k_ap=k_ap, ssl=ssl: e.matmul(Sb[:, ssl], k_ap, q_ap, start=True, stop=False),
                 reads=[kb, qb], writes=[(Sb, sq_)])
            p.op("pe", lambda e, Sb=Sb, w=w, ssl=ssl: e.matmul(Sb[:, ssl], KAw[0:4, hw * 128:(hw + 1) * 128],
                                                              QAw[0:4, (hw * 2 + w) * 128:(hw * 2 + w + 1) * 128], start=False, stop=False),
                 reads=[KAw, QAw], writes=[(Sb, sq_)])
            p.op("pe", lambda e, Sb=Sb, mi=mi, ssl=ssl: e.matmul(Sb[:, ssl], identb[:, :], masks[:, mi * 128:(mi + 1) * 128], start=False, stop=True),
                 reads=[identb, masks], writes=[(Sb, sq_)])
            pb = c.pb[(c.npb // 4) % 3]
            pq_ = c.npb % 4
            c.npb += 1
            psl = slice(pq_ * 128, (pq_ + 1) * 128)
            p.op("act", lambda e, Sb=Sb, pb=pb, ssl=ssl, psl=psl: e.activation(pb[:, psl], Sb[:, ssl], AF.Exp),
                 reads=[(Sb, sq_)], writes=[(pb, pq_)])
            first, last = (n == 0), (n == len(items) - 1)

            def s2(pb=pb, v_ap=v_ap, first=first, last=last, vb=vb, psl=psl, pq_=pq_):
                p.op("pe", lambda e: e.matmul(O_ap, v_ap, pb[:, psl], start=first, stop=last),
                     reads=[vb, (pb, pq_)], writes=[O])
                p.op("pe", lambda e: e.matmul(L_ap, c.ones[:, :], pb[:, psl], start=first, stop=last),
                     reads=[c.ones, (pb, pq_)], writes=[L])
            pend.append(s2)
            flush(3)

    nb = 0
    for jl in range(nslots):
        for g in range(ngroups):
            d = DIL_D[g]
            hw = jl * 3 + g
            q_, k_, v_ = qs[nb % 2], ks[nb % 2], vs[nb % 2]
            nb += 1
            c.kq_bufs = (k_, q_, v_)
            hdA = g * 4 + jl
            hdB = g * 4 + 2 + jl
            c.load_fm(q_, dq, 1536, hdA * 128, hdB * 128)
            c.load_fm(k_, dk, 1536, hdA * 128, hdB * 128)
            nbk = S // (128 * d)
            for r in range(d):
                for (dst_, hd_, nm) in ((v_, hdA, "A"), (c.candv, hdB, "B")):
                    dvb = dv[(hd_ * 128) // 512]
                    cc_ = (hd_ * 128) % 512
                    src = dvb.t[:, cc_:cc_ + 128].rearrange("(b p r) e -> r p b e", p=128, r=d)[r]
                    p.dma("sp", dst_[:, r * nbk:(r + 1) * nbk, :], src, reads=[dvb], writes=[(dst_, r)], chan="ldv%s_%d" % (nm, r % 4))
            c.blend(lambda v_=v_: v_[:, :, :], lambda: c.candv[:, :, :], v_, c.candv)
            bat = min(4, nbk)
            for r in range(d):
                for b0 in range(0, nbk, bat):
                    O = c.psO[c.nO % 2]
                    L = c.psL[c.nO % 2]
                    c.nO += 1
                    for jb in range(bat):
                        b = b0 + jb
                        col = lambda bb: slice(r + d * 128 * bb, r + d * 128 * bb + d * 127 + 1, d)
                        q_ap = q_[:, col(b)]
                        k_aps = [k_[:, col(b - 1)] if b > 0 else None, k_[:, col(b)]]
                        v_aps = [v_[:, r * nbk + b - 1, :] if b > 0 else None, v_[:, r * nbk + b, :]]
                        win_tile(q_ap, k_aps, v_aps, hw, 1, O[:, jb * 128:(jb + 1) * 128], L[:, jb * 128:(jb + 1) * 128], O, L)
                    flush(0)
                    n = bat * 128
                    tsl = slice(r + d * 128 * b0, r + d * 128 * b0 + d * (bat * 128 - 1) + 1, d)
                    if g == 0:
                        p.op("act", lambda e, O=O, tsl=tsl, n=n: e.activation(Oacc[:, tsl], O[:, 0:n], AF.Copy), reads=[O], writes=[Oacc])
                        p.op("act", lambda e, L=L, tsl=tsl, n=n: e.activation(Lacc[:, tsl], L[:, 0:n], AF.Copy), reads=[L], writes=[Lacc])
                    else:
                        p.op("dve", lambda e, O=O, tsl=tsl, n=n: e.tensor_tensor(Oacc[:, tsl], O[:, 0:n], Oacc[:, tsl], ALU.add), reads=[O, Oacc], writes=[Oacc])
                        p.op("dve", lambda e, L=L, tsl=tsl, n=n: e.tensor_tensor(Lacc[:, tsl], L[:, 0:n], Lacc[:, tsl], ALU.add), reads=[L, Lacc], writes=[Lacc])
        for ch in range(S // 512):
            cs = slice(ch * 512, (ch + 1) * 512)
            ob = c.ob[c.nob % 2]
            c.nob += 1
            p.op("dve", lambda e, cs=cs: e.reciprocal(c.rl[:, :], Lacc[:, cs]), reads=[Lacc], writes=[c.rl])
            p.op("dve", lambda e, cs=cs, ob=ob: e.tensor_tensor(ob[:, :], Oacc[:, cs], c.rl[:, :], ALU.mult), reads=[Oacc, c.rl], writes=[ob])
            p.dma("sp", atT[0].t[jl * 128:(jl + 1) * 128, cs], ob[:, :], reads=[ob], writes=[(atT[0], (jl, ch))], chan=ob.name)
    piece_done(0)
    if nswa:
        k_ = ks[nb % 2]
        v_ = vs[nb % 2]
        c.load_fm(k_, sk2, 256, 0, 128)
        for jj in range(2):
            p.dma("sp", v_[:, :, jj * 64:(jj + 1) * 64], sv[0].t[:, 0:64].rearrange("(t p) e -> p t e", p=128), reads=[sv[0]], writes=[(v_, jj)], chan="ldvA_%d" % jj)
            p.dma("sp", c.candv[:, :, jj * 64:(jj + 1) * 64], sv[0].t[:, 64:128].rearrange("(t p) e -> p t e", p=128), reads=[sv[0]], writes=[(c.candv, jj)], chan="ldvB_%d" % jj)
        c.blend(lambda: v_[:, :, :], lambda: c.candv[:, :, :], v_, c.candv)
        for hl in range(nswa):
            if hl % 2 == 0:
                q_ = qs[(hl // 2) % 2]
                c.load_fm(q_, sq, 1024, (hl // 2) * 128, 512 + (hl // 2) * 128)
            c.kq_bufs = (k_, q_, v_)
            po = (hl % 2) * 64
            hw = 6 + hl
            for b0 in range(0, S // 128, 4):
                O = c.psO[c.nO % 2]
                L = c.psL[c.nO % 2]
                c.nO += 1
                for jb in range(4):
                    b = b0 + jb
                    col = lambda bb: slice(128 * bb, 128 * (bb + 1))
                    q_ap = q_[po:po + 64, col(b)]
                    k_aps = [k_[po:po + 64, col(b - 1)] if b > 0 else None, k_[po:po + 64, col(b)]]
                    v_aps = [v_[:, b - 1, :] if b > 0 else None, v_[:, b, :]]
                    win_tile(q_ap, k_aps, v_aps, hw, 2, O[:, jb * 128:(jb + 1) * 128], L[:, jb * 128:(jb + 1) * 128], O, L)
                flush(0)
                cs = slice(b0 * 128, b0 * 128 + 512)
                ob = c.ob[c.nob % 2]
                c.nob += 1
                p.op("dve", lambda e, L=L, hl=hl, po=po: e.tensor_scalar(c.rl[po:po + 64, :], L[po:po + 64, :], es[po:po + 64, hl:hl + 1], None, ALU.add),
                     reads=[L, es], writes=[c.rl])
                p.op("dve", lambda e, po=po: e.reciprocal(c.rl[po:po + 64, :], c.rl[po:po + 64, :]), reads=[c.rl], writes=[c.rl])
                p.op("dve", lambda e, O=O, ob=ob, po=po: e.tensor_tensor(ob[po:po + 64, :], O[po:po + 64, :], c.rl[po:po + 64, :], ALU.mult),
                     reads=[O, c.rl], writes=[ob])
                ro_ = 256 + hl * 64
                p.dma("sp", atT[ro_ // 256].t[ro_ % 256:ro_ % 256 + 64, cs], ob[po:po + 64, :], reads=[ob], writes=[(atT[ro_ // 256], ("s", hl, b0))], chan=ob.name)
            if hl % 4 == 3:
                piece_done(1 + hl // 4)

import numpy as np
import ml_dtypes

def gain_layout(g):
    return np.ascontiguousarray(np.asarray(g, np.float32).reshape(-1, 128).T)

def prep_w_in_odd(w):
    cd = 1536
    dq, dk, dv = w[:, :cd], w[:, cd:2 * cd], w[:, 2 * cd:3 * cd]
    o = 3 * cd
    sq = w[:, o:o + 1024]
    sk = w[:, o + 1024:o + 1152]
    sv = w[:, o + 1152:o + 1280]
    sk2 = np.concatenate([sk[:, :64], sk[:, :64], sk[:, 64:], sk[:, 64:]], axis=1)
    return np.ascontiguousarray(np.concatenate([dq, dk, dv, sq, sk2, sv], axis=1))

def _swap(a):
    n = a.shape[1] // 64
    b = a.reshape(a.shape[0], n, 2, 32)[:, :, ::-1, :]
    return b.reshape(a.shape[0], n * 64)

def prep_w_in_even(w):
    qlat, kvlat, kpe = w[:, :512], w[:, 512:1024], w[:, 1024:1088]
    rest = w[:, 1088:]
    krx = np.concatenate([kpe, kpe], axis=1)
    krs = _swap(krx)
    return np.ascontiguousarray(np.concatenate([qlat, kvlat, krx, krs, rest], axis=1))

def prep_w_uq(w):
    w3 = w.reshape(512, 8, 192)
    nope = w3[:, :, :128].reshape(512, 1024)
    rope = w3[:, :, 128:].reshape(512, 512)
    rsw = _swap(rope)
    parts = [nope]
    for j in range(4):
        parts += [rope[:, j * 128:(j + 1) * 128], rsw[:, j * 128:(j + 1) * 128]]
    return np.ascontiguousarray(np.concatenate(parts, axis=1))

def prep_w_ukv(w):
    w3 = w.reshape(512, 8, 256)
    return np.ascontiguousarray(np.concatenate([w3[:, :, :128].reshape(512, 1024), w3[:, :, 128:].reshape(512, 1024)], axis=1))

def rope_tables(S=4096):
    half = 32
    inv = (10000.0 ** (-np.arange(half, dtype=np.float32) / half)).astype(np.float32)
    ang = np.arange(S, dtype=np.float32)[None, :] * inv[:, None]
    cos, sin = np.cos(ang).astype(np.float32), np.sin(ang).astype(np.float32)
    C = np.concatenate([cos, cos, cos, cos], axis=0)
    Sg = np.concatenate([-sin, sin, -sin, sin], axis=0)
    return np.ascontiguousarray(C), np.ascontiguousarray(Sg)

def make_KA(S=4096):
    KA = np.zeros((36, S), np.float32)
    s = np.arange(S)
    KA[s // 256, s] = 1.0
    KA[32] = 1.0
    KA[33] = 1.0
    KA[34] = 128.0 * (s // 128)
    KA[35] = s % 128
    return KA.astype(ml_dtypes.bfloat16)

def make_QAc(heads, nheads_total=8, S=4096):
    t = np.arange(S)
    out = np.zeros((len(heads), 4, S), np.float32)
    for i, h in enumerate(heads):
        slope = 2.0 ** (-8.0 * (h + 1) / nheads_total)
        out[i, 0] = -slope * 128.0 * (t // 128)
        out[i, 1] = -slope * (t % 128)
        out[i, 2] = slope
        out[i, 3] = slope
    return out.astype(ml_dtypes.bfloat16)

def _hilo(x):
    hi = np.float32(x).astype(ml_dtypes.bfloat16)
    lo = (np.float32(x) - hi.astype(np.float32)).astype(ml_dtypes.bfloat16)
    return hi, lo

def make_win_consts(hh):
    cs = []
    for jl in range(2):
        for g in range(3):
            hd = g * 4 + (2 * hh + jl)
            slope = 2.0 ** (-8.0 * (hd + 1) / 12)
            cs.append(slope * (1, 4, 16)[g])
    for hl in range(8):
        hq = 8 * hh + hl
        cs.append(2.0 ** (-8.0 * (hq + 1) / 16))
    KAw = np.zeros((4, 14, 128), np.float32)
    QAw = np.zeros((4, 14, 2, 128), np.float32)
    sl = np.arange(128, dtype=np.float32)
    for i, cval in enumerate(cs):
        hi, lo = _hilo(cval)
        hi, lo = np.float32(hi), np.float32(lo)
        KAw[0, i] = hi; KAw[1, i] = lo; KAw[2, i] = sl; KAw[3, i] = sl
        for w, off in ((0, 128.0), (1, 0.0)):
            QAw[0, i, w] = -(sl + off); QAw[1, i, w] = -(sl + off); QAw[2, i, w] = hi; QAw[3, i, w] = lo
    s_ = np.arange(128)[:, None]; t_ = np.arange(128)[None, :]
    NEGV = -30000.0
    m_own = np.where(t_ >= s_, 0.0, NEGV)
    m_p128 = np.where(t_ <= s_, 0.0, NEGV)
    m_p127 = np.where(t_ < s_, 0.0, NEGV)
    masks = np.concatenate([m_own, m_p128, m_p127], axis=1)
    bf = ml_dtypes.bfloat16
    return (KAw.reshape(4, -1).astype(bf), QAw.reshape(4, -1).astype(bf), masks.astype(bf), np.eye(128, dtype=np.float32).astype(bf))


def make_cmask():
    pp = np.arange(128)[:, None]
    col = np.arange(512)[None, :]
    return np.concatenate([np.where(col - 128 * j - pp >= 0, 0.0, -30000.0).astype(np.float32) for j in range(4)], axis=1).astype(ml_dtypes.bfloat16)


PAIRS = [[0, 1], [2, 3], [4, 5], [6, 7]]
NLAYER = 4


def build_fused(NLAYER=NLAYER, with_attn=True, ncoll=99):
    nc = bass.Bass("TRN2", target_bir_lowering=False)
    p = Prog(nc)
    p.arena_init(208896)
    ps = [p.psum("ps%d" % i, [128, 512], F32) for i in range(8)]
    ones = p.sb("C", "ones", [128, 128], BF16)
    gains = p.sb("C", "gains", [128, 64], F32)
    msel = p.sb("C", "msel", [128, 2], F32)
    p.op("dve", lambda e: e.memset(ones[:, :], 1.0), writes=[ones])
    di = lambda n, sh, dt=F32: p.dram(n, sh, dt, kind="ExternalInput")
    T = TCORE
    mseld = di("msel", [128, 2])
    p.dma("sp", msel[:, :], mseld[:, :], reads=[mseld], writes=[msel])
    lc = LCtx(p, ps, ones, gains)
    ac = ACtx(p, ps, ones, msel)
    cmaskd = di("cmask", [128, 4 * 512], BF16)
    xT = di("xT", [D, T])
    cosd = di("cosd", [128, T]); sind = di("sind", [128, T])
    KAd = di("KA", [36, S], BF16); QAcd = di("QAc", [4, 4, S], BF16); identd = di("ident", [128, 128])
    KAwd = di("KAw", [4, 14 * 128], BF16); QAwd = di("QAw", [4, 14 * 2 * 128], BF16)
    maskd = di("masks", [128, 3 * 128], BF16); identbd = di("identb", [128, 128], BF16)
    yo = p.dram("yo", [D, T], F32, kind="ExternalOutput")
    atG = None
    x_cur = xT
    for i in range(NLAYER + 1):
        io = {"xT": x_cur, "msel": msel}
        if i > 0:
            l = i - 1
            natt = 16 if l % 2 == 0 else 12
            io.update(atG=atG, w_out=di("w_out%d" % l, [natt * 128, D]), g_mlp=di("g_mlp%d" % l, [128, 16]),
                      w_up=di("w_up%d" % l, [D, DFF]), w_dn=di("w_dn%d" % l, [DFF, D]))
        if i < NLAYER:
            even = i % 2 == 0
            io.update(g_att=di("g_att%d" % i, [128, 16]), w_in=di("w_in%d" % i, [D, W_IN_EVEN_COLS if even else W_IN_ODD_COLS]))
            shapes = OUT_SHAPES_EVEN if even else OUT_SHAPES_ODD
            outs = {}
            for nm, (kind, n) in shapes.items():
                ch = min(512, n)
                bufs = [p.dram("o%d_%s_%d" % (i, nm, j), [ch, T] if kind == "fm" else [T, ch], BF16) for j in range(n // ch)]
                outs[nm] = Split(kind, bufs, ch)
            io["outs"] = outs
            if even:
                io.update(g_q=di("g_q%d" % i, [128, 4]), g_kv=di("g_kv%d" % i, [128, 4]), w_uq=di("w_uq%d" % i, [512, 2048]),
                          w_ukv=di("w_ukv%d" % i, [512, 2048]), cosd=cosd, sind=sind)
            io["xo"] = p.dram("x%d" % (i + 1), [D, T], F32)
        else:
            io.update(g_fin=di("g_fin", [128, 16]), yo=yo)
        emit_L(p, lc, i, io, last=(i == NLAYER))
        if i == NLAYER:
            break
        x_cur = io["xo"]
        p.barrier()
        gath = {}
        for nm, (kind, n) in shapes.items():
            gl = []
            for j, ob_ in enumerate(outs[nm].bufs):
                ch = outs[nm].chunk
                gb = p.dram("g%d_%s_%d" % (i, nm, j), [2 * ch, T] if kind == "fm" else [2 * T, ch], BF16)
                p.coll("AllGather", PAIRS, ob_, gb, chan="cc_%s_%d" % (nm, j))
                gl.append(gb)
            gath[nm] = gl
        nat = 4 if even else 3
        at = [p.dram("at%d_%d" % (i, j), [256, S], BF16) for j in range(nat)]
        atG = [p.dram("atG%d_%d" % (i, j), [512, S], BF16) for j in range(nat)]
        aio = dict(gath)
        aio["atT"] = at
        p.dma("sp", ac.cmask[:, :], cmaskd[:, :], reads=[cmaskd], writes=[ac.cmask])
        p.dma("sp", ac.identb[:, :], identbd[:, :], reads=[identbd], writes=[ac.identb])

        def piece_done(j, at=at, atG=atG):
            p.coll("AllGather", PAIRS, at[j], atG[j], chan="cc_at_%d" % j)
        if even:
            aio.update(KA=KAd, QAc=QAcd, ident=identd)
            emit_A_even(p, ac, aio, piece_done=piece_done)
        else:
            aio.update(KAw=KAwd, QAw=QAwd, masks=maskd, identb=identbd, sinks=di("sinks%d" % i, [128, 8]))
            emit_A_odd(p, ac, aio, piece_done=piece_done)
        p.barrier()
    st = p.emit()
    st["arena"] = dict(p.arena_off)
    p.close()
    return nc, st


_FUSED = {}


def kernel(x, attn_norm, mlp_norm, w_up, w_down, ev_w_in, ev_q_norm, ev_w_uq, ev_kv_norm,
           ev_w_ukv, ev_w_out, od_w_in, od_sinks, od_w_out, final_norm):
    from concourse.bass_utils import run_bass_kernel_spmd
    A = lambda a: np.ascontiguousarray(np.asarray(a))
    x = A(x)
    B, SS, DD = x.shape
    cores = list(range(8))
    if "nc" not in _FUSED:
        _FUSED["nc"] = build_fused()[0]
    nc = _FUSED["nc"]
    C_, S_ = rope_tables()
    common = dict(KA=make_KA(), ident=np.eye(128, dtype=np.float32), g_fin=gain_layout(final_norm), cmask=make_cmask())
    for l in range(NLAYER):
        common["g_mlp%d" % l] = gain_layout(mlp_norm[l])
        common["w_up%d" % l] = A(w_up[l])
        common["w_dn%d" % l] = A(w_down[l])
        common["g_att%d" % l] = gain_layout(attn_norm[l])
        if l % 2 == 0:
            j = l // 2
            common["w_out%d" % l] = A(ev_w_out[j])
            common["w_in%d" % l] = prep_w_in_even(A(ev_w_in[j]))
            common["g_q%d" % l] = gain_layout(ev_q_norm[j])
            common["g_kv%d" % l] = gain_layout(ev_kv_norm[j])
            common["w_uq%d" % l] = prep_w_uq(A(ev_w_uq[j]))
            common["w_ukv%d" % l] = prep_w_ukv(A(ev_w_ukv[j]))
        else:
            j = l // 2
            common["w_out%d" % l] = A(od_w_out[j])
            common["w_in%d" % l] = prep_w_in_odd(A(od_w_in[j]))
    in_maps = []
    for c in cores:
        b, r = c // 2, c % 2
        m = dict(common)
        m["xT"] = A(x[b, r * 2048:(r + 1) * 2048, :].T)
        m["cosd"] = A(C_[:, r * 2048:(r + 1) * 2048])
        m["sind"] = A(S_[:, r * 2048:(r + 1) * 2048])
        m["msel"] = A(np.tile(np.array([[1.0 - r, float(r)]], np.float32), (128, 1)))
        m["QAc"] = make_QAc(list(range(4 * r, 4 * r + 4)))
        KAw, QAw, masks, identb = make_win_consts(r)
        m.update(KAw=KAw, QAw=QAw, masks=masks, identb=identb)
        for l in (1, 3):
            sk = np.asarray(od_sinks[l // 2], np.float32)
            m["sinks%d" % l] = A(np.tile(sk[None, 8 * r:8 * r + 8], (128, 1)))
        in_maps.append(m)
    res = run_bass_kernel_spmd(nc, in_maps, core_ids=cores).results
    out = np.empty((B, SS, DD), np.float32)
    for c in cores:
        out[c // 2, (c % 2) * 2048:(c % 2 + 1) * 2048, :] = res[c]["yo"].T
    return out
```
